# Optimizing a Trainium2 kernel written in Bass

```python
import jax, jax.numpy as jnp
from jax import lax
import numpy as np

D_MODEL = 1024
BATCH = 2
SEQ = 8192
DEPTH = 2

GRID_W = 64
CTX_LEN = 256

HEAD = 64
D_RWKV = D_MODEL // 2
H_RWKV = D_RWKV // HEAD
D_CONV = D_MODEL // 4
D_FOUR = D_MODEL - D_RWKV - D_CONV
FOUR_GROUP = 64
G_FOUR = D_FOUR // FOUR_GROUP
LORA_W = 64
LORA_A = 64
LORA_G = 128
D_RWKV_IN = 3 * D_RWKV + 2 * LORA_W + 2 * LORA_A + LORA_G
RWKV_SPLITS = (D_RWKV, 2 * D_RWKV, 3 * D_RWKV, 3 * D_RWKV + LORA_W, 3 * D_RWKV + 2 * LORA_W,
               3 * D_RWKV + 2 * LORA_W + LORA_A, 3 * D_RWKV + 2 * LORA_W + 2 * LORA_A)
D_IN = D_RWKV_IN + 3 * D_CONV + D_FOUR
CONV_W = 3
N_GROUPS = 4
EXPERTS_PER_GROUP = 8
N_EXPERTS = N_GROUPS * EXPERTS_PER_GROUP
TOP_K = 2
D_EXPERT = D_MODEL // 2
MOE_BLOCK = 128
RMS_EPS = 1e-6
GN_EPS = 64e-5

kernel_name = "hybrid_rwkv7_shortconv_fourier_hmoe_dit"


def rmsnorm(x, gain):
    xf = x.astype(jnp.float32)
    y = xf * lax.rsqrt(jnp.mean(xf * xf, axis=-1, keepdims=True) + RMS_EPS)
    return (y * gain).astype(x.dtype)


def to_heads(t):
    return t.reshape(t.shape[0], t.shape[1], H_RWKV, HEAD)


def grid_shift(z):
    b, length, ch = z.shape
    rows = length // GRID_W
    zg = z.reshape(b, rows, GRID_W, ch // 4, 4)
    left = jnp.pad(zg[:, :, :-1, :, 0], ((0, 0), (0, 0), (1, 0), (0, 0)))
    right = jnp.pad(zg[:, :, 1:, :, 1], ((0, 0), (0, 0), (0, 1), (0, 0)))
    up = jnp.pad(zg[:, :-1, :, :, 2], ((0, 0), (1, 0), (0, 0), (0, 0)))
    down = jnp.pad(zg[:, 1:, :, :, 3], ((0, 0), (0, 1), (0, 0), (0, 0)))
    return jnp.stack([left, right, up, down], axis=-1).reshape(b, length, ch)


def seq_shift(z):
    b, length, ch = z.shape
    zs = z.reshape(b, length, ch // 2, 2)
    prev = jnp.pad(zs[:, :-1, :, 0], ((0, 0), (1, 0), (0, 0)))
    nxt = jnp.pad(zs[:, 1:, :, 1], ((0, 0), (0, 1), (0, 0)))
    return jnp.stack([prev, nxt], axis=-1).reshape(b, length, ch)


def rwkv_streams(z, decay_w0, decay_w2, iclr_a0, iclr_a2, gate_g2, k_k, k_a):
    zf = z.astype(jnp.float32)
    r, k, v, lw_f, lw_b, la_f, la_b, lg = jnp.split(zf, RWKV_SPLITS, axis=-1)
    g = jax.nn.sigmoid(lg) @ gate_g2
    kk = to_heads(k * k_k)
    kk = kk / jnp.maximum(jnp.sqrt(jnp.sum(kk * kk, axis=-1, keepdims=True)), 1e-12)
    per_dir = []
    for dn, (lw, la) in enumerate(((lw_f, la_f), (lw_b, la_b))):
        w_log = -jax.nn.softplus(-(decay_w0[dn] + jnp.tanh(lw) @ decay_w2[dn])) - 0.5
        decay = jnp.exp(-jnp.exp(w_log))
        a = jax.nn.sigmoid(iclr_a0[dn] + la @ iclr_a2[dn])
        k_dir = k * (1.0 + (a - 1.0) * k_a)
        per_dir.append((to_heads(decay), to_heads(k_dir), to_heads(a)))
    return to_heads(r), to_heads(v), kk, g, per_dir


def wkv7_scan(s0, r, decay, k, v, kk, a, reverse, readout):
    def step(s, inp):
        r_t, d_t, k_t, v_t, kk_t, a_t = inp
        s_kk = jnp.einsum('bhvk,bhk->bhv', s, kk_t)
        s = (s * d_t[:, :, None, :]
             - s_kk[..., None] * (kk_t * a_t)[:, :, None, :]
             + v_t[..., None] * k_t[:, :, None, :])
        y_t = jnp.einsum('bhvk,bhk->bhv', s, r_t) if readout else None
        return s, y_t
    xs = tuple(jnp.swapaxes(t, 0, 1) for t in (r, decay, k, v, kk, a))
    s, y = lax.scan(step, s0, xs, reverse=reverse)
    return s, (jnp.swapaxes(y, 0, 1) if readout else None)


def rwkv_readout(ys, r, k_dirs, v, g, r_k, gn_w, gn_b):
    b, length = r.shape[0], r.shape[1]
    y = ys[0] + ys[1]
    mu = jnp.mean(y, axis=-1, keepdims=True)
    var = jnp.mean(jnp.square(y - mu), axis=-1, keepdims=True)
    yn = ((y - mu) * lax.rsqrt(var + GN_EPS)).reshape(b, length, D_RWKV) * gn_w + gn_b
    bonus = (jnp.sum(r * k_dirs[0] * r_k, axis=-1, keepdims=True) * v
             + jnp.sum(r * k_dirs[1] * r_k, axis=-1, keepdims=True) * v)
    return (yn + bonus.reshape(b, length, D_RWKV)) * g


def gated_short_conv(p, conv_w, conv_gain):
    bg, cg, h = jnp.split(p, 3, axis=-1)
    z = cg * h
    prev = jnp.pad(z[:, :-1], ((0, 0), (1, 0), (0, 0)))
    nxt = jnp.pad(z[:, 1:], ((0, 0), (0, 1), (0, 0)))
    y = bg * (conv_w[0] * prev + conv_w[1] * z + conv_w[2] * nxt)
    return rmsnorm(y, conv_gain)


def fourier_mix(p, four_gain):
    b, length, _ = p.shape
    f = p.astype(jnp.float32).reshape(b, length, G_FOUR, FOUR_GROUP)
    y = jnp.fft.fft2(f, axes=(1, 3), norm='ortho').real.reshape(b, length, D_FOUR)
    return rmsnorm(y.astype(p.dtype), four_gain)


def assemble(p, o_rwkv, conv_w, conv_gain, four_gain, w_out):
    cut1, cut2 = D_RWKV_IN, D_RWKV_IN + 3 * D_CONV
    conv_o = gated_short_conv(p[..., cut1:cut2], conv_w, conv_gain)
    four_o = fourier_mix(p[..., cut2:], four_gain)
    return jnp.concatenate([o_rwkv.astype(p.dtype), conv_o, four_o], axis=-1) @ w_out


def token_mixers(hx, hc, w_in, mu_shift, decay_w0, decay_w2, iclr_a0, iclr_a2, gate_g2, k_k, k_a, r_k,
                 gn_w, gn_b, conv_w, conv_gain, four_gain, w_out, need_ctx):
    px = hx @ w_in
    pc = hc @ w_in
    zx = px[..., :D_RWKV_IN]
    zx = zx + (grid_shift(zx) - zx) * mu_shift
    zc = pc[..., :D_RWKV_IN]
    zc = zc + (seq_shift(zc) - zc) * mu_shift
    rx, vx, kkx, gx, dx = rwkv_streams(zx, decay_w0, decay_w2, iclr_a0, iclr_a2, gate_g2, k_k, k_a)
    rc, vc, kkc, gc, dc = rwkv_streams(zc, decay_w0, decay_w2, iclr_a0, iclr_a2, gate_g2, k_k, k_a)
    s0 = jnp.zeros((hx.shape[0], H_RWKV, HEAD, HEAD), jnp.float32)
    ys_x, ys_c = [], []
    for dn in range(2):
        rev = dn == 1
        dec_c, k_c, a_c = dc[dn]
        s_ctx, y_c = wkv7_scan(s0, rc, dec_c, k_c, vc, kkc, a_c, rev, need_ctx)
        dec_x, k_x, a_x = dx[dn]
        _, y_x = wkv7_scan(s_ctx, rx, dec_x, k_x, vx, kkx, a_x, rev, True)
        ys_x.append(y_x)
        ys_c.append(y_c)
    ox = rwkv_readout(ys_x, rx, [d[1] for d in dx], vx, gx, r_k, gn_w, gn_b)
    out_x = assemble(px, ox, conv_w, conv_gain, four_gain, w_out)
    if not need_ctx:
        return out_x, None
    oc = rwkv_readout(ys_c, rc, [d[1] for d in dc], vc, gc, r_k, gn_w, gn_b)
    out_c = assemble(pc, oc, conv_w, conv_gain, four_gain, w_out)
    return out_x, out_c


def hier_moe(h, router_g_w, router_g_b, router_e_w, router_e_b, exp_gate, exp_up, exp_down):
    n, d = h.shape
    hf = h.astype(jnp.float32)
    pg = jax.nn.softmax(hf @ router_g_w.astype(jnp.float32) + router_g_b, axis=-1)
    pg_top, grp = lax.top_k(pg, 1)
    le = (hf @ router_e_w.astype(jnp.float32) + router_e_b).reshape(n, N_GROUPS, EXPERTS_PER_GROUP)
    le_g = jnp.take_along_axis(le, grp[:, :, None], axis=1)[:, 0]
    top_l, top_i = lax.top_k(le_g, TOP_K)
    gates = pg_top * jax.nn.softmax(top_l, axis=-1)
    eid = grp * EXPERTS_PER_GROUP + top_i
    m = n * TOP_K
    flat_e = eid.reshape(m)
    flat_t = jnp.repeat(jnp.arange(n, dtype=jnp.int32), TOP_K)
    flat_w = gates.reshape(m)
    order = jnp.argsort(flat_e)
    se = flat_e[order]
    counts = jnp.bincount(flat_e, length=N_EXPERTS)
    start = jnp.cumsum(counts) - counts
    pcounts = (counts + MOE_BLOCK - 1) // MOE_BLOCK * MOE_BLOCK
    pend = jnp.cumsum(pcounts)
    pstart = pend - pcounts
    dest = pstart[se] + jnp.arange(m, dtype=jnp.int32) - start[se]
    n_blocks = -(-m // MOE_BLOCK) + N_EXPERTS
    cap = n_blocks * MOE_BLOCK
    buf_t = jnp.full((cap,), n, jnp.int32).at[dest].set(flat_t[order])
    buf_w = jnp.zeros((cap,), jnp.float32).at[dest].set(flat_w[order])
    block_e = jnp.minimum(jnp.searchsorted(pend, jnp.arange(n_blocks) * MOE_BLOCK, side='right'),
                          N_EXPERTS - 1)
    h_pad = jnp.concatenate([h, jnp.zeros((1, d), h.dtype)], axis=0)
    xb = h_pad[buf_t].reshape(n_blocks, MOE_BLOCK, d)

    def expert_block(args):
        xblk, e = args
        return (jax.nn.silu(xblk @ exp_gate[e]) * (xblk @ exp_up[e])) @ exp_down[e]

    yb = lax.map(expert_block, (xb, block_e)).reshape(cap, d)
    out = jnp.zeros((n + 1, d), jnp.float32).at[buf_t].add(yb.astype(jnp.float32) * buf_w[:, None])
    return out[:n].astype(h.dtype)


def setup_inputs(seed: int = 0) -> dict:
    key = jax.random.key(seed)
    k = jax.random.split(key, 32)
    f32 = jnp.float32
    L, D = DEPTH, D_MODEL

    def nrm(i, shape, s):
        return jax.random.normal(k[i], shape, f32) * s

    return {
        "x": nrm(0, (BATCH, SEQ, D), 1.0),
        "c": nrm(1, (BATCH, D), 1.0),
        "ctx": nrm(2, (BATCH, CTX_LEN, D), 1.0),
        "c_ctx": nrm(3, (D,), 1.0),
        "ada_w": nrm(4, (L, D, 6 * D), 0.5 * D ** -0.5),
        "ada_b": nrm(5, (L, 6 * D), 0.02),
        "norm1_g": 1.0 + nrm(6, (L, D), 0.05),
        "norm2_g": 1.0 + nrm(7, (L, D), 0.05),
        "w_in": nrm(8, (L, D, D_IN), D ** -0.5),
        "mu_shift": jax.random.uniform(k[9], (L, D_RWKV_IN), f32),
        "decay_w0": jax.random.uniform(k[10], (L, 2, D_RWKV), f32, -6.0, -1.0),
        "decay_w2": nrm(11, (L, 2, LORA_W, D_RWKV), 0.5 * LORA_W ** -0.5),
        "iclr_a0": nrm(12, (L, 2, D_RWKV), 0.1),
        "iclr_a2": nrm(13, (L, 2, LORA_A, D_RWKV), 0.5 * LORA_A ** -0.5),
        "gate_g2": nrm(14, (L, LORA_G, D_RWKV), LORA_G ** -0.5),
        "k_k": 0.85 + nrm(15, (L, D_RWKV), 0.05),
        "k_a": 1.0 + nrm(16, (L, D_RWKV), 0.05),
        "r_k": nrm(17, (L, H_RWKV, HEAD), 0.1),
        "gn_w": 1.0 + nrm(18, (L, D_RWKV), 0.05),
        "gn_b": nrm(19, (L, D_RWKV), 0.02),
        "conv_w": nrm(20, (L, CONV_W, D_CONV), 0.5),
        "conv_gain": 1.0 + nrm(21, (L, D_CONV), 0.05),
        "four_gain": 1.0 + nrm(22, (L, D_FOUR), 0.05),
        "w_out": nrm(23, (L, D, D), D ** -0.5),
        "router_g_w": nrm(24, (L, D, N_GROUPS), D ** -0.5),
        "router_g_b": nrm(25, (L, N_GROUPS), 0.01),
        "router_e_w": nrm(26, (L, D, N_EXPERTS), D ** -0.5),
        "router_e_b": nrm(27, (L, N_EXPERTS), 0.01),
        "exp_gate": nrm(28, (L, N_EXPERTS, D, D_EXPERT), D ** -0.5),
        "exp_up": nrm(29, (L, N_EXPERTS, D, D_EXPERT), D ** -0.5),
        "exp_down": nrm(30, (L, N_EXPERTS, D_EXPERT, D), D_EXPERT ** -0.5),
        "final_g": 1.0 + nrm(31, (D,), 0.05),
    }


def reference(x, c, ctx, c_ctx, ada_w, ada_b, norm1_g, norm2_g, w_in, mu_shift, decay_w0, decay_w2,
              iclr_a0, iclr_a2, gate_g2, k_k, k_a, r_k, gn_w, gn_b, conv_w, conv_gain, four_gain, w_out,
              router_g_w, router_g_b, router_e_w, router_e_b, exp_gate, exp_up, exp_down, final_g):
    bsz, seq, d = x.shape
    n_lat = bsz * seq
    for l in range(DEPTH):
        last = l == DEPTH - 1
        mod_x = jax.nn.silu(c) @ ada_w[l] + ada_b[l]
        mod_c = jax.nn.silu(c_ctx) @ ada_w[l] + ada_b[l]
        sh1x, sc1x, g1x, sh2x, sc2x, g2x = jnp.split(mod_x[:, None, :], 6, axis=-1)
        sh1c, sc1c, g1c, sh2c, sc2c, g2c = jnp.split(mod_c, 6)
        hx = rmsnorm(x, norm1_g[l]) * (1.0 + sc1x) + sh1x
        hc = rmsnorm(ctx, norm1_g[l]) * (1.0 + sc1c) + sh1c
        mx, mc = token_mixers(hx, hc, w_in[l], mu_shift[l], decay_w0[l], decay_w2[l], iclr_a0[l], iclr_a2[l],
                              gate_g2[l], k_k[l], k_a[l], r_k[l], gn_w[l], gn_b[l], conv_w[l], conv_gain[l],
                              four_gain[l], w_out[l], not last)
        x = x + g1x * mx
        hx = rmsnorm(x, norm2_g[l]) * (1.0 + sc2x) + sh2x
        moe_args = (router_g_w[l], router_g_b[l], router_e_w[l], router_e_b[l], exp_gate[l], exp_up[l], exp_down[l])
        if last:
            x = x + g2x * hier_moe(hx.reshape(n_lat, d), *moe_args).reshape(x.shape)
        else:
            ctx = ctx + g1c * mc
            hc = rmsnorm(ctx, norm2_g[l]) * (1.0 + sc2c) + sh2c
            y = hier_moe(jnp.concatenate([hx.reshape(n_lat, d), hc.reshape(-1, d)], axis=0), *moe_args)
            x = x + g2x * y[:n_lat].reshape(x.shape)
            ctx = ctx + g2c * y[n_lat:].reshape(ctx.shape)
    return rmsnorm(x, final_g)
```

```python
import contextlib
import numpy as np
import concourse.bass as bass
import concourse.mybir as mybir
from concourse.bass_utils import run_bass_kernel_spmd

F32 = mybir.dt.float32
BF16 = mybir.dt.bfloat16
ALU = mybir.AluOpType
AF = mybir.ActivationFunctionType
AX = mybir.AxisListType


class Prog:
    def __init__(self, nc, stack):
        self.nc = nc
        self.stack = stack
        self.engs = {"pe": nc.tensor, "act": nc.scalar, "dve": nc.vector, "pool": nc.gpsimd, "sp": nc.sync}
        self.sem = {}
        self.seq = {}
        for name in ["pe", "act", "dve", "pool"]:
            self.sem[name] = stack.enter_context(nc.semaphore("s_" + name))
            self.seq[name] = 0
        self.known = {e: {} for e in self.engs}
        self.last_write = {}
        self.reads_since = {}
        self.n_instr = 0

    def _wait(self, eng, src, val):
        if val <= 0:
            return
        if self.known[eng].get(src, 0) >= val:
            return
        self.engs[eng].wait_ge(self.sem[src], val)
        self.known[eng][src] = val
        self.n_instr += 1

    def _deps(self, eng, me, reads, writes):
        need = {}

        def add(ev, same_ok):
            if ev is None:
                return
            src, val = ev
            if src == me and same_ok:
                return
            if need.get(src, 0) < val:
                need[src] = val

        for k in reads:
            add(self.last_write.get(k), False)
        for k in writes:
            add(self.last_write.get(k), me == "pe")
            for src, val in self.reads_since.get(k, {}).items():
                add((src, val), False)
        for src, val in need.items():
            self._wait(eng, src, val)

    def _record(self, me, val, reads, writes):
        for k in reads:
            d = self.reads_since.setdefault(k, {})
            if d.get(me, 0) < val:
                d[me] = val
        for k in writes:
            self.last_write[k] = (me, val)
            self.reads_since[k] = {}

    def op(self, eng, fn, *args, reads=(), writes=(), **kw):
        self._deps(eng, eng, reads, writes)
        ins = getattr(self.engs[eng], fn)(*args, **kw)
        ins.then_inc(self.sem[eng], 1)
        self.seq[eng] += 1
        self.n_instr += 1
        self._record(eng, self.seq[eng], reads, writes)
        return ins

    def dma(self, q, out, in_, reads=(), writes=(), sem=None, **kw):
        if sem is None:
            sem = writes[0]
        me = ("dma", sem)
        if me not in self.sem:
            self.sem[me] = self.stack.enter_context(self.nc.semaphore("sd%d" % len(self.sem)))
            self.seq[me] = 0
        self._wait(q, me, self.seq[me])
        self._deps(q, me, reads, writes)
        ins = self.engs[q].dma_start(out=out, in_=in_, **kw)
        ins.then_inc(self.sem[me], 16)
        self.seq[me] += 16
        self.n_instr += 1
        self._record(me, self.seq[me], reads, writes)
        return ins

    def finish(self, keys):
        for k in keys:
            ev = self.last_write.get(k)
            if ev is not None:
                self._wait("sp", ev[0], ev[1])


def new_prog():
    nc = bass.Bass("TRN2", target_bir_lowering=False)
    stack = contextlib.ExitStack()
    P = Prog(nc, stack)
    return nc, stack, P


def sb(P, name, shape, dt=F32):
    return P.stack.enter_context(P.nc.sbuf_tensor(name, list(shape), dt))


def ps(P, name, shape, dt=F32):
    return P.stack.enter_context(P.nc.psum_tensor(name, list(shape), dt))


def dram_in(nc, name, shape, dt=F32):
    return nc.dram_tensor(name, list(shape), dt, kind="ExternalInput").ap()


def dram_out(nc, name, shape, dt=F32):
    return nc.dram_tensor(name, list(shape), dt, kind="ExternalOutput").ap()


SCAN_CH = 8
YCH = 256


def build_scan(T):
    nc, stack, P = new_prog()
    with stack:
        rows_d = [dram_in(nc, f"rows{d}", [2, T, 448]) for d in range(2)]
        dcol_d = [dram_in(nc, f"dcol{d}", [128, T]) for d in range(2)]
        rpad_d = [dram_in(nc, f"rpad{d}", [128, T, 2]) for d in range(2)]
        ident_d = dram_in(nc, "ident", [128, 128])
        y_d = [dram_out(nc, f"y{d}", [64, T, 2]) for d in range(2)]

        ident = sb(P, "ident_sb", [128, 128])
        P.dma("sp", ident[:], ident_d, writes=["ident"])
        NCOL = 1056
        assert T % SCAN_CH == 0
        st = []
        for d in range(2):
            s = dict(
                rows=[sb(P, f"rows_sb{d}_{i}", [2, SCAN_CH, 448]) for i in range(2)],
                dcol=[sb(P, f"dcol_sb{d}_{i}", [128, NCOL]) for i in range(2)],
                rpad=[sb(P, f"rpad_sb{d}_{i}", [128, NCOL, 2]) for i in range(2)],
                H=[sb(P, f"H{d}_{i}", [128, 64]) for i in range(2)],
                Pt=[sb(P, f"Pt{d}_{i}", [128, 128]) for i in range(3)],
                psP=[ps(P, f"psP{d}_{i}", [128, 128])[:] for i in range(2)],
                psH=[ps(P, f"psH{d}", [128, 64])[:]] * 2,
                psY=[ps(P, f"psY{d}", [64, YCH, 2])] * 2,
                ysb=[sb(P, f"ysb{d}_{i}", [64, YCH, 2]) for i in range(2)],
            )
            st.append(s)
            P.op("dve", "memset", s["H"][1][:], 0.0, writes=[("H", d, 1)])

        def load_rows(d, c):
            s = st[d]
            b = c % 2
            q = "sp" if d == 0 else "pool"
            P.dma(q, s["rows"][b][:], rows_d[d][:, c * SCAN_CH:(c + 1) * SCAN_CH, :], writes=[("rows", d, b)])

        def load_cols(d, c):
            s = st[d]
            b = c % 2
            n = min(NCOL, T - c * NCOL)
            q = "sp" if d == 0 else "pool"
            P.dma(q, s["dcol"][b][:, :n], dcol_d[d][:, c * NCOL:c * NCOL + n], writes=[("dcol", d, b)])
            P.dma(q, s["rpad"][b][:, :n, :], rpad_d[d][:, c * NCOL:c * NCOL + n, :], writes=[("rpad", d, b)])

        def mmP(d, t):
            s = st[d]
            rb = (t // SCAN_CH) % 2
            j = t % SCAN_CH
            rows = s["rows"][rb]
            P.op("pe", "matmul", s["psP"][t % 2], rows[:, j, 0:128], rows[:, j, 128:256],
                 start=True, stop=True,
                 reads=[("rows", d, rb)], writes=[("psP", d, t % 2)])

        def step(d, t):
            s = st[d]
            rb = (t // SCAN_CH) % 2
            j = t % SCAN_CH
            cb = (t // NCOL) % 2
            jc = t % NCOL
            rows = s["rows"][rb]
            hp, hn = (t + 1) % 2, t % 2
            P.op("dve", "scalar_tensor_tensor", s["Pt"][t % 3][:], ident[:], s["dcol"][cb][:, jc:jc + 1],
                 s["psP"][t % 2], ALU.mult, ALU.add,
                 reads=["ident", ("dcol", d, cb), ("psP", d, t % 2)], writes=[("Pt", d, t % 3)])
            P.op("pe", "matmul", s["psH"][t % 2], s["Pt"][t % 3][:], s["H"][hp][:], start=True, stop=False,
                 reads=[("Pt", d, t % 3), ("H", d, hp)], writes=[("psH", d)])
            P.op("pe", "matmul", s["psH"][t % 2], rows[:, j, 256:384], rows[:, j, 384:448], start=False, stop=True,
                 reads=[("rows", d, rb)], writes=[("psH", d)])
            P.op("act", "copy", s["H"][hn][:], s["psH"][t % 2],
                 reads=[("psH", d)], writes=[("H", d, hn)])

        def yout(d, t):
            s = st[d]
            cb = (t // NCOL) % 2
            jc = t % NCOL
            yb = (t // YCH) % 2
            jy = t % YCH
            P.op("pe", "matmul", s["psY"][yb][:, jy, :], s["H"][t % 2][:], s["rpad"][cb][:, jc, :],
                 start=True, stop=True,
                 reads=[("H", d, t % 2), ("rpad", d, cb)], writes=[("psY", d)])
            if jy == YCH - 1 or t == T - 1:
                n = jy + 1
                t0 = t - jy
                P.op("dve", "tensor_copy", s["ysb"][yb][:, :n, :], s["psY"][yb][:, :n, :],
                     reads=[("psY", d)], writes=[("ysb", d, yb)])
                P.dma("sp", y_d[d][:, t0:t0 + n, :], s["ysb"][yb][:, :n, :],
                      reads=[("ysb", d, yb)], writes=[("yout", d)])

        for d in range(2):
            load_rows(d, 0)
            load_cols(d, 0)
        for d in range(2):
            mmP(d, 0)
        for t in range(T):
            for d in range(2):
                if t >= 1:
                    yout(d, t - 1)
            for d in range(2):
                if t % SCAN_CH == 0 and (t // SCAN_CH + 1) * SCAN_CH < T:
                    load_rows(d, t // SCAN_CH + 1)
                if t % NCOL == 0 and (t // NCOL + 1) * NCOL < T:
                    load_cols(d, t // NCOL + 1)
            for d in range(2):
                if t + 1 < T:
                    mmP(d, t + 1)
                step(d, t)
        for d in range(2):
            yout(d, T - 1)
        P.finish([("yout", 0), ("yout", 1)])
    return nc, P


def build_mods():
    nc, stack, P = new_prog()
    with stack:
        cT_d = dram_in(nc, "cT", [128, 8, 3])
        w_d = dram_in(nc, "adaw", [2, 8, 128, 768])
        b_d = dram_in(nc, "adab", [2, 1, 768])
        o_d = dram_out(nc, "mod", [2, 3, 768])
        cT = sb(P, "cT_sb", [128, 8, 3])
        sg = sb(P, "sg", [128, 8, 3])
        sl = sb(P, "sl", [128, 8, 3])
        ones = sb(P, "ones1", [1, 3])
        P.dma("sp", cT[:], cT_d, writes=["cT"])
        P.op("act", "activation", sg[:], cT[:], AF.Sigmoid, reads=["cT"], writes=["sg"])
        P.op("dve", "tensor_tensor", sl[:], sg[:], cT[:], ALU.mult, reads=["sg", "cT"], writes=["sl"])
        P.op("dve", "memset", ones[:], 1.0, writes=["ones"])
        for l in range(2):
            w = sb(P, f"w{l}", [128, 8, 768])
            b = sb(P, f"b{l}", [1, 768])
            o = sb(P, f"o{l}", [3, 768])
            for k in range(8):
                P.dma("sp" if k % 2 == 0 else "pool", w[:, k, :], w_d[l, k], writes=[("w", l, k)])
            P.dma("sp", b[:], b_d[l], writes=[("b", l)])
            for h in range(2):
                pt = ps(P, f"ps{l}_{h}", [3, 384])
                cs = slice(h * 384, (h + 1) * 384)
                for k in range(8):
                    P.op("pe", "matmul", pt[:], sl[:, k, :], w[:, k, cs], start=(k == 0), stop=False,
                         reads=["sl", ("w", l, k)], writes=[("ps", l, h)])
                P.op("pe", "matmul", pt[:], ones[:], b[:, cs], start=False, stop=True,
                     reads=["ones", ("b", l)], writes=[("ps", l, h)])
                P.op("dve", "tensor_copy", o[:, cs], pt[:], reads=[("ps", l, h)], writes=[("o", l, h)])
            P.dma("sp", o_d[l], o[:], reads=[("o", l, 0), ("o", l, 1)], writes=[("out", l)])
        P.finish([("out", 0), ("out", 1)])
    return nc, P


def run_prog(nc, in_maps, n=8):
    res = run_bass_kernel_spmd(nc, in_maps, core_ids=list(range(n)))
    return res.results


def host_mods(inputs):
    cvec = np.concatenate([inputs["c"], inputs["c_ctx"][None, :]], axis=0).astype(np.float32)
    cT = np.ascontiguousarray(cvec.T.reshape(8, 128, 3).transpose(1, 0, 2))
    nc, P = build_mods()
    maps = []
    for i in range(8):
        cs = slice(i * 768, (i + 1) * 768)
        maps.append({
            "cT": cT,
            "adaw": np.ascontiguousarray(inputs["ada_w"][:, :, cs].reshape(2, 8, 128, 768)),
            "adab": np.ascontiguousarray(inputs["ada_b"][:, None, cs]),
        })
    res = run_prog(nc, maps)
    return np.concatenate([r["mod"] for r in res], axis=2)


NTOK = 2112
CHUNKS = [(0, 512, 0), (512, 512, 0), (1024, 512, 0), (1536, 512, 0), (2048, 64, 1)]
RMS_EPS = 1e-6


def col_layout(v):
    v = np.asarray(v, np.float32)
    lead = v.shape[:-1]
    return np.ascontiguousarray(np.moveaxis(v.reshape(lead + (8, 128)), -1, 0))


def emit_rmsnorm_mod(P, tag, xT, c0, n, gcol, scol, hT, ones, sq, pss, rstd, tmp, h0=None, hkey=None):
    if h0 is None:
        h0 = c0
    for k in range(8):
        P.op("act", "activation", sq[:, :n], xT[:, k, c0:c0 + n], AF.Square,
             reads=[(tag, "x", k)], writes=[(tag, "sq")])
        P.op("pe", "matmul", pss[:, :n], ones[:], sq[:, :n], start=(k == 0), stop=(k == 7),
             reads=[(tag, "sq"), "ones"], writes=[(tag, "pss")])
    P.op("dve", "tensor_scalar", rstd[:, :n], pss[:, :n], 1.0 / 1024.0, RMS_EPS, ALU.mult, ALU.add,
         reads=[(tag, "pss")], writes=[(tag, "rstd")])
    P.op("act", "activation", rstd[:, :n], rstd[:, :n], AF.Sqrt,
         reads=[(tag, "rstd")], writes=[(tag, "rstd")])
    P.op("dve", "reciprocal", rstd[:, :n], rstd[:, :n],
         reads=[(tag, "rstd")], writes=[(tag, "rstd")])
    for k in range(8):
        P.op("dve", "tensor_tensor", tmp[:, :n], xT[:, k, c0:c0 + n], rstd[:, :n], ALU.mult,
             reads=[(tag, "x", k), (tag, "rstd")], writes=[(tag, "tmp")])
        P.op("dve", "tensor_scalar", hT[:, k, h0:h0 + n], tmp[:, :n], gcol[:, k:k + 1], scol[:, k:k + 1],
             ALU.mult, ALU.add, reads=[(tag, "tmp"), (tag, "cols")], writes=[hkey if hkey is not None else (tag, "h", k)])


D_IN = 2944
NCH_IN = 23


def build_proj():
    nc, stack, P = new_prog()
    with stack:
        xT_d = dram_in(nc, "xT", [8, 128, NTOK])
        w_d = dram_in(nc, "w", [8, 128, D_IN])
        cols_d = dram_in(nc, "cols", [128, 2, 3, 8])
        o_d = dram_out(nc, "pT", [NCH_IN, 128, NTOK])
        xT = sb(P, "xT_sb", [128, 8, NTOK])
        hT = sb(P, "hT_sb", [128, 8, NTOK], BF16)
        w = sb(P, "w_sb", [128, 8, D_IN], BF16)
        cols = sb(P, "cols_sb", [128, 2, 3, 8])
        geff = sb(P, "geff", [128, 2, 8])
        ones = sb(P, "ones", [128, 128])
        sq = sb(P, "sq", [128, 512])
        tmp = sb(P, "tmp", [128, 512])
        rstd = sb(P, "rstd", [128, 512])
        pss = ps(P, "pss", [128, 512])
        pso = [ps(P, f"pso{i}", [128, 512]) for i in range(4)]
        osb = [sb(P, f"osb{i}", [128, 512]) for i in range(4)]
        P.op("dve", "memset", ones[:], 1.0, writes=["ones"])
        P.dma("sp", cols[:], cols_d, writes=["cols"])
        for k in range(8):
            P.dma("sp", xT[:, k, :], xT_d[k], writes=[("B", "x", k)])
            P.dma("pool", w[:, k, :], w_d[k], writes=[("w", k)])
        for s in range(2):
            P.op("dve", "scalar_tensor_tensor", geff[:, s, :], cols[:, s, 1, :], 1.0, cols[:, s, 0, :],
                 ALU.add, ALU.mult, reads=["cols"], writes=[("B", "cols")])
        cnt = 0
        for (c0, n, seg) in CHUNKS:
            emit_rmsnorm_mod(P, "B", xT, c0, n, geff[:, seg, :], cols[:, seg, 2, :], hT, ones, sq, pss, rstd, tmp)
            for j in range(NCH_IN):
                b = cnt % 4
                cnt += 1
                for k in range(8):
                    P.op("pe", "matmul", pso[b][:, :n], w[:, k, j * 128:(j + 1) * 128], hT[:, k, c0:c0 + n],
                         start=(k == 0), stop=(k == 7),
                         reads=[("w", k), ("B", "h", k)], writes=[("pso", b)])
                eng = "act" if j % 2 == 0 else "dve"
                if eng == "act":
                    P.op("act", "copy", osb[b][:, :n], pso[b][:, :n], reads=[("pso", b)], writes=[("osb", b)])
                else:
                    P.op("dve", "tensor_copy", osb[b][:, :n], pso[b][:, :n], reads=[("pso", b)], writes=[("osb", b)])
                P.dma("sp", o_d[j, :, c0:c0 + n], osb[b][:, :n], reads=[("osb", b)], writes=[("out", b)])
        P.finish([("out", b) for b in range(4)])
    return nc, P


def shard_tokens_T(xlat, xctx):
    outs = []
    fl = xlat.reshape(16384, -1)
    fc = xctx.reshape(512, -1)
    for i in range(8):
        t = np.concatenate([fl[i * 2048:(i + 1) * 2048], fc[i * 64:(i + 1) * 64]], axis=0)
        outs.append(np.ascontiguousarray(t.T))
    return outs


def unshard_tokens_T(outs):
    lat = np.concatenate([o[:, :2048].T for o in outs], axis=0)
    ctx = np.concatenate([o[:, 2048:].T for o in outs], axis=0)
    D = lat.shape[1]
    return lat.reshape(2, 8192, D), ctx.reshape(2, 256, D)


def mod_cols(mod_l, kinds, i, gain):
    b = i // 4
    out = np.zeros((128, 2, 3, 8), np.float32)
    for s, row in enumerate((b, 2)):
        out[:, s, 0, :] = col_layout(gain)
        out[:, s, 1, :] = col_layout(mod_l[row, kinds[0] * 1024:(kinds[0] + 1) * 1024])
        out[:, s, 2, :] = col_layout(mod_l[row, kinds[1] * 1024:(kinds[1] + 1) * 1024])
    return out


_CACHE = {}


def cached(name, fn, *a):
    key = (name,) + a
    if key not in _CACHE:
        _CACHE[key] = fn(*a)
    return _CACHE[key]


def host_proj(xlat, xctx, mod_l, gain, w_in_l):
    nc, P = cached("proj", build_proj)
    xs = shard_tokens_T(xlat, xctx)
    wl = np.ascontiguousarray(w_in_l.reshape(8, 128, D_IN))
    maps = []
    for i in range(8):
        maps.append({"xT": xs[i].reshape(8, 128, NTOK), "w": wl, "cols": mod_cols(mod_l, (1, 0), i, gain)})
    res = run_prog(nc, maps)
    return unshard_tokens_T([r["pT"].reshape(D_IN, NTOK) for r in res])


TSEQ = 8448
C_OUTS = ["r", "v", "kk", "nb0", "nb1", "kd0", "kd1", "d0", "d1", "g", "bonus"]
DEC_SCALE = float(-np.exp(-0.5))


def build_streams():
    nc, stack, P = new_prog()
    with stack:
        z_d = dram_in(nc, "zin", [9, 128, TSEQ])
        pc_d = dram_in(nc, "pc", [128, 16])
        mk_d = dram_in(nc, "masks", [128, 6])
        wl_d = dram_in(nc, "wl", [4, 128, 128])
        g2_d = dram_in(nc, "g2", [2, 128, 128])
        ob_d = dram_in(nc, "onesbd", [128, 128])
        outs = {n: dram_out(nc, "o_" + n, [128, TSEQ]) for n in C_OUTS}

        pc = sb(P, "pc_sb", [128, 16])
        mk = sb(P, "mk_sb", [128, 6])
        wl = sb(P, "wl_sb", [128, 4, 128])
        g2 = sb(P, "g2_sb", [128, 2, 128])
        ob = sb(P, "ob_sb", [128, 128])
        cs = sb(P, "cshift", [128, 9, 7])
        omka = sb(P, "omka", [128, 1])
        P.dma("sp", pc[:], pc_d, writes=["pc"])
        P.dma("sp", mk[:], mk_d, writes=["mk"])
        for i in range(4):
            P.dma("sp", wl[:, i, :], wl_d[i], writes=[("wl", i)])
        for i in range(2):
            P.dma("sp", g2[:, i, :], g2_d[i], writes=[("g2", i)])
        P.dma("sp", ob[:], ob_d, writes=["ob"])
        for s in range(9):
            P.op("dve", "tensor_scalar", cs[:, s, 0:1], pc[:, s:s + 1], -1.0, 1.0, ALU.mult, ALU.add,
                 reads=["pc"], writes=[("cs", s)])
            P.op("dve", "tensor_scalar", cs[:, s, 1:7], mk[:], pc[:, s:s + 1], None, ALU.mult,
                 reads=["pc", "mk"], writes=[("cs", s)])
        P.op("dve", "tensor_scalar", omka[:], pc[:, 10:11], -1.0, 1.0, ALU.mult, ALU.add, reads=["pc"], writes=["omka"])
        KK, KA, RK, W0F, W0B, A0F, A0B = 9, 10, 11, 12, 13, 14, 15

        NB = 1024
        zb = [sb(P, f"zb{i}", [128, 18, 64]) for i in range(2)]
        acc = {n: sb(P, "acc_" + n, [128, NB]) for n in ["r", "k", "v", "lw0", "lw1", "la0", "la1", "lg0", "lg1"]}
        t1 = sb(P, "t1", [128, NB])
        t2 = sb(P, "t2", [128, NB])
        kk = sb(P, "kk", [128, NB])
        kds = sb(P, "kds", [128, NB])
        ot = [sb(P, f"ot{i}", [128, NB]) for i in range(4)]
        pm = [ps(P, f"pm{i}", [128, 512]) for i in range(4)]
        names = ["r", "k", "v", "lw0", "lw1", "la0", "la1", "lg0", "lg1"]
        state = {"zb": 0, "ot": 0, "pm": 0}

        def out_tile():
            i = state["ot"] % 4
            state["ot"] += 1
            return ot[i], ("ot", i)

        def psum_tile():
            i = state["pm"] % 4
            state["pm"] += 1
            return pm[i], ("pm", i)

        def store(name, tile, key, t0, n):
            P.dma("sp", outs[name][:, t0:t0 + n], tile[:, :n], reads=[key], writes=[("out", name)], sem=("st", key))

        def shift(s, t0, n, ctx, j):
            bi = state["zb"] % 2
            state["zb"] += 1
            z = zb[bi]
            zk = ("zb", bi)
            a = acc[names[s]]
            ak = ("acc", s)
            q = "sp" if s % 2 == 0 else "pool"
            if ctx:
                zf = z[:].rearrange("p a b -> p (a b)")
                P.dma(q, zf[:, 0:256], z_d[s, :, 0:256], writes=[zk])
                P.op("dve", "tensor_scalar", a[:, :256], zf[:, 0:256], cs[:, s, 0:1], None, ALU.mult,
                     reads=[zk, ("cs", s)], writes=[ak])
                P.op("dve", "scalar_tensor_tensor", a[:, 1:256], zf[:, 0:255], cs[:, s, 5:6], a[:, 1:256],
                     ALU.mult, ALU.add, reads=[zk, ("cs", s), ak], writes=[ak])
                P.op("dve", "scalar_tensor_tensor", a[:, 0:255], zf[:, 1:256], cs[:, s, 6:7], a[:, 0:255],
                     ALU.mult, ALU.add, reads=[zk, ("cs", s), ak], writes=[ak])
                return
            r0 = 16 * j - 1
            lo = 1 if j == 0 else 0
            hi = 17 if j == 7 else 18
            if j == 0:
                P.op("dve", "memset", z[:, 0, :], 0.0, writes=[zk])
            if j == 7:
                P.op("dve", "memset", z[:, 17, :], 0.0, writes=[zk])
            src = z_d[s, :, 256 + (r0 + lo) * 64:256 + (r0 + hi) * 64].rearrange("p (a b) -> p a b", b=64)
            P.dma(q, z[:, lo:hi, :], src, writes=[zk])
            a3 = a[:].rearrange("p (a b) -> p a b", b=64)
            eng = "dve"
            P.op(eng, "tensor_scalar", a3[:, :, :], z[:, 1:17, :], cs[:, s, 0:1], None, ALU.mult,
                 reads=[zk, ("cs", s)], writes=[ak])
            for (dst, srcv, c) in ((a3[:, :, 1:64], z[:, 1:17, 0:63], 1), (a3[:, :, 0:63], z[:, 1:17, 1:64], 2),
                                   (a3[:, :, :], z[:, 0:16, :], 3), (a3[:, :, :], z[:, 2:18, :], 4)):
                P.op(eng, "scalar_tensor_tensor", dst, srcv, cs[:, s, c:c + 1], dst, ALU.mult, ALU.add,
                     reads=[zk, ("cs", s), ak], writes=[ak])

        chunks = [(0, 256, True, 0)] + [(256 + 1024 * j, 1024, False, j) for j in range(8)]
        for (t0, n, ctx, j) in chunks:
            for s in range(9):
                shift(s, t0, n, ctx, j)
            ncc = [(c0, min(512, n - c0)) for c0 in range(0, n, 512)]
            store("r", acc["r"], ("acc", 0), t0, n)
            store("v", acc["v"], ("acc", 2), t0, n)
            P.op("dve", "tensor_scalar", t1[:, :n], acc["k"][:, :n], pc[:, KK:KK + 1], None, ALU.mult,
                 reads=[("acc", 1), "pc"], writes=["t1"])
            P.op("act", "activation", t2[:, :n], t1[:, :n], AF.Square, reads=["t1"], writes=["t2"])
            for (c0, m) in ncc:
                pt, pk = psum_tile()
                P.op("pe", "matmul", pt[:, :m], ob[:], t2[:, c0:c0 + m], start=True, stop=True,
                     reads=["ob", "t2"], writes=[pk])
                P.op("act", "activation", kk[:, c0:c0 + m], pt[:, :m], AF.Sqrt, reads=[pk], writes=["kk"])
            P.op("dve", "tensor_scalar", kk[:, :n], kk[:, :n], 1e-12, None, ALU.max, reads=["kk"], writes=["kk"])
            P.op("dve", "reciprocal", kk[:, :n], kk[:, :n], reads=["kk"], writes=["kk"])
            P.op("dve", "tensor_tensor", kk[:, :n], kk[:, :n], t1[:, :n], ALU.mult, reads=["kk", "t1"], writes=["kk"])
            store("kk", kk, "kk", t0, n)
            for dn in range(2):
                P.op("act", "activation", t1[:, :n], acc[f"lw{dn}"][:, :n], AF.Tanh,
                     reads=[("acc", 3 + dn)], writes=["t1"])
                o, okey = out_tile()
                for (c0, m) in ncc:
                    pt, pk = psum_tile()
                    P.op("pe", "matmul", pt[:, :m], wl[:, dn, :], t1[:, c0:c0 + m], start=True, stop=True,
                         reads=[("wl", dn), "t1"], writes=[pk])
                    P.op("act", "activation", o[:, c0:c0 + m], pt[:, :m], AF.Sigmoid,
                         bias=pc[:, W0F + dn:W0F + dn + 1], reads=[pk, "pc"], writes=[okey])
                P.op("act", "activation", o[:, :n], o[:, :n], AF.Exp, scale=DEC_SCALE, reads=[okey], writes=[okey])
                store(f"d{dn}", o, okey, t0, n)
                for (c0, m) in ncc:
                    pt, pk = psum_tile()
                    P.op("pe", "matmul", pt[:, :m], wl[:, 2 + dn, :], acc[f"la{dn}"][:, c0:c0 + m], start=True, stop=True,
                         reads=[("wl", 2 + dn), ("acc", 5 + dn)], writes=[pk])
                    P.op("act", "activation", t2[:, c0:c0 + m], pt[:, :m], AF.Sigmoid,
                         bias=pc[:, A0F + dn:A0F + dn + 1], reads=[pk, "pc"], writes=["t2"])
                o, okey = out_tile()
                P.op("dve", "scalar_tensor_tensor", o[:, :n], t2[:, :n], -1.0, kk[:, :n], ALU.mult, ALU.mult,
                     reads=["t2", "kk"], writes=[okey])
                store(f"nb{dn}", o, okey, t0, n)
                o, okey = out_tile()
                P.op("dve", "tensor_scalar", t2[:, :n], t2[:, :n], pc[:, KA:KA + 1], omka[:, 0:1], ALU.mult, ALU.add,
                     reads=["t2", "pc", "omka"], writes=["t2"])
                P.op("dve", "tensor_tensor", o[:, :n], t2[:, :n], acc["k"][:, :n], ALU.mult,
                     reads=["t2", ("acc", 1)], writes=[okey])
                store(f"kd{dn}", o, okey, t0, n)
                if dn == 0:
                    P.op("dve", "tensor_copy", kds[:, :n], o[:, :n], reads=[okey], writes=["kds"])
                else:
                    P.op("dve", "tensor_tensor", kds[:, :n], kds[:, :n], o[:, :n], ALU.add,
                         reads=[okey, "kds"], writes=["kds"])
            for b in range(2):
                P.op("act", "activation", acc[f"lg{b}"][:, :n], acc[f"lg{b}"][:, :n], AF.Sigmoid,
                     reads=[("acc", 7 + b)], writes=[("acc", 7 + b)])
            o, okey = out_tile()
            for (c0, m) in ncc:
                pt, pk = psum_tile()
                for b in range(2):
                    P.op("pe", "matmul", pt[:, :m], g2[:, b, :], acc[f"lg{b}"][:, c0:c0 + m], start=(b == 0), stop=(b == 1),
                         reads=[("g2", b), ("acc", 7 + b)], writes=[pk])
                P.op("act", "copy", o[:, c0:c0 + m], pt[:, :m], reads=[pk], writes=[okey])
            store("g", o, okey, t0, n)
            P.op("dve", "scalar_tensor_tensor", t1[:, :n], acc["r"][:, :n], pc[:, RK:RK + 1], kds[:, :n], ALU.mult, ALU.mult,
                 reads=[("acc", 0), "pc", "kds"], writes=["t1"])
            o, okey = out_tile()
            for (c0, m) in ncc:
                pt, pk = psum_tile()
                P.op("pe", "matmul", pt[:, :m], ob[:], t1[:, c0:c0 + m], start=True, stop=True,
                     reads=["ob", "t1"], writes=[pk])
                P.op("dve", "tensor_tensor", o[:, c0:c0 + m], pt[:, :m], acc["v"][:, c0:c0 + m], ALU.mult,
                     reads=[pk, ("acc", 2)], writes=[okey])
            store("bonus", o, okey, t0, n)
        P.finish([("out", n) for n in C_OUTS])
    return nc, P


def host_streams(plat, pctx, inp, l):
    nc, P = cached("streams", build_streams)
    pall = np.concatenate([pctx, plat], axis=1)
    masks = np.zeros((128, 6), np.float32)
    p = np.arange(128)
    for s in range(4):
        masks[:, s] = (p % 4 == s)
    masks[:, 4] = (p % 2 == 0)
    masks[:, 5] = (p % 2 == 1)
    onesbd = np.zeros((128, 128), np.float32)
    onesbd[:64, :64] = 1
    onesbd[64:, 64:] = 1

    def bh(v):
        return np.concatenate([v, v])

    def bd(m):
        o = np.zeros((128, 128), np.float32)
        o[:64, :64] = m
        o[64:, 64:] = m
        return o

    mu = inp["mu_shift"][l]
    maps = []
    for h in range(8):
        hs = slice(h * 64, (h + 1) * 64)
        zin = np.empty((9, 128, TSEQ), np.float32)
        for s, base in enumerate((0, 512, 1024)):
            zin[s] = pall[:, :, base + h * 64: base + (h + 1) * 64].transpose(0, 2, 1).reshape(128, TSEQ)
        for s, base in enumerate((1536, 1600, 1664, 1728)):
            zin[3 + s] = pall[:, :, base:base + 64].transpose(0, 2, 1).reshape(128, TSEQ)
        for b in range(2):
            zin[7 + b] = pall[b, :, 1792:1920].T
        pc = np.zeros((128, 16), np.float32)
        for s, base in enumerate((0, 512, 1024)):
            pc[:, s] = bh(mu[base + h * 64: base + (h + 1) * 64])
        for s, base in enumerate((1536, 1600, 1664, 1728)):
            pc[:, 3 + s] = bh(mu[base:base + 64])
        pc[:, 7] = mu[1792:1920]
        pc[:, 8] = mu[1792:1920]
        pc[:, 9] = bh(inp["k_k"][l][hs])
        pc[:, 10] = bh(inp["k_a"][l][hs])
        pc[:, 11] = bh(inp["r_k"][l][h])
        pc[:, 12] = bh(inp["decay_w0"][l][0][hs])
        pc[:, 13] = bh(inp["decay_w0"][l][1][hs])
        pc[:, 14] = bh(inp["iclr_a0"][l][0][hs])
        pc[:, 15] = bh(inp["iclr_a0"][l][1][hs])
        wl = np.stack([bd(inp["decay_w2"][l][0][:, hs]), bd(inp["decay_w2"][l][1][:, hs]),
                       bd(inp["iclr_a2"][l][0][:, hs]), bd(inp["iclr_a2"][l][1][:, hs])])
        g2 = np.zeros((2, 128, 128), np.float32)
        g2[0, :, :64] = inp["gate_g2"][l][:, hs]
        g2[1, :, 64:] = inp["gate_g2"][l][:, hs]
        maps.append({"zin": zin, "pc": pc, "masks": masks, "wl": wl, "g2": g2, "onesbd": onesbd})
    res = run_prog(nc, maps)
    return [{n: r["o_" + n] for n in C_OUTS} for r in res]


IDX_F = np.arange(TSEQ)
IDX_B = np.concatenate([np.arange(255, -1, -1), 256 + np.arange(8191, -1, -1)])


def host_scan(streams):
    T = TSEQ
    nc, P = cached("scan", build_scan, T)
    ident = np.eye(128, dtype=np.float32)
    maps = []
    for h in range(8):
        s = streams[h]
        m = {"ident": ident}
        for dn, idx in enumerate((IDX_F, IDX_B)):
            rows = np.zeros((2, T, 448), np.float32)
            rpad = np.zeros((128, T, 2), np.float32)
            for c in range(2):
                ps_ = slice(c * 64, (c + 1) * 64)
                rows[c, :, c * 64:(c + 1) * 64] = s["kk"][ps_][:, idx].T
                rows[c, :, 128 + c * 64:128 + (c + 1) * 64] = s[f"nb{dn}"][ps_][:, idx].T
                rows[c, :, 256 + c * 64:256 + (c + 1) * 64] = s[f"kd{dn}"][ps_][:, idx].T
                rows[c, :, 384:448] = s["v"][ps_][:, idx].T
                rpad[ps_, :, c] = s["r"][ps_][:, idx]
            m[f"rows{dn}"] = rows
            m[f"dcol{dn}"] = np.ascontiguousarray(s[f"d{dn}"][:, idx])
            m[f"rpad{dn}"] = rpad
        maps.append(m)
    res = run_prog(nc, maps)
    out = []
    for h in range(8):
        ys = []
        for dn, idx in enumerate((IDX_F, IDX_B)):
            y = res[h][f"y{dn}"]
            yfm = np.empty((128, T), np.float32)
            for c in range(2):
                yfm[c * 64:(c + 1) * 64][:, idx] = y[:, :, c]
            ys.append(yfm)
        out.append(tuple(ys))
    return out


GN_EPS = 64e-5


def build_readout():
    nc, stack, P = new_prog()
    with stack:
        yf_d = dram_in(nc, "yf", [128, TSEQ])
        yb_d = dram_in(nc, "yb", [128, TSEQ])
        bo_d = dram_in(nc, "bonus", [128, TSEQ])
        g_d = dram_in(nc, "g", [128, TSEQ])
        pc_d = dram_in(nc, "pc", [128, 2])
        ob_d = dram_in(nc, "obd64", [128, 128])
        o_d = dram_out(nc, "o", [128, TSEQ])
        pc = sb(P, "pc_sb", [128, 2])
        ob = sb(P, "ob_sb", [128, 128])
        P.dma("sp", pc[:], pc_d, writes=["pc"])
        P.dma("sp", ob[:], ob_d, writes=["ob"])
        NB = 512
        nbuf = 2
        bufs = []
        for i in range(nbuf):
            bufs.append({n: sb(P, f"{n}{i}", [128, NB]) for n in ["yf", "yb", "bo", "g", "t", "sq"]})
        pmu = [ps(P, f"pmu{i}", [128, NB]) for i in range(2)]
        pvar = [ps(P, f"pvar{i}", [128, NB]) for i in range(2)]
        nchunks = (TSEQ + NB - 1) // NB
        for c in range(nchunks):
            i = c % nbuf
            B = bufs[i]
            t0 = c * NB
            n = min(NB, TSEQ - t0)
            k = lambda nm: (nm, i)
            P.dma("sp", B["yf"][:, :n], yf_d[:, t0:t0 + n], writes=[k("yf")])
            P.dma("pool", B["yb"][:, :n], yb_d[:, t0:t0 + n], writes=[k("yb")])
            P.dma("sp", B["bo"][:, :n], bo_d[:, t0:t0 + n], writes=[k("bo")])
            P.dma("pool", B["g"][:, :n], g_d[:, t0:t0 + n], writes=[k("g")])
            P.op("dve", "tensor_tensor", B["yf"][:, :n], B["yf"][:, :n], B["yb"][:, :n], ALU.add,
                 reads=[k("yf"), k("yb")], writes=[k("yf")])
            P.op("pe", "matmul", pmu[i][:, :n], ob[:], B["yf"][:, :n], start=True, stop=True,
                 reads=["ob", k("yf")], writes=[k("pmu")])
            P.op("dve", "tensor_tensor", B["t"][:, :n], B["yf"][:, :n], pmu[i][:, :n], ALU.subtract,
                 reads=[k("yf"), k("pmu")], writes=[k("t")])
            P.op("act", "activation", B["sq"][:, :n], B["t"][:, :n], AF.Square, reads=[k("t")], writes=[k("sq")])
            P.op("pe", "matmul", pvar[i][:, :n], ob[:], B["sq"][:, :n], start=True, stop=True,
                 reads=["ob", k("sq")], writes=[k("pvar")])
            P.op("dve", "tensor_scalar", B["sq"][:, :n], pvar[i][:, :n], GN_EPS, None, ALU.add,
                 reads=[k("pvar")], writes=[k("sq")])
            P.op("act", "activation", B["sq"][:, :n], B["sq"][:, :n], AF.Sqrt, reads=[k("sq")], writes=[k("sq")])
            P.op("dve", "reciprocal", B["sq"][:, :n], B["sq"][:, :n], reads=[k("sq")], writes=[k("sq")])
            P.op("dve", "tensor_tensor", B["t"][:, :n], B["t"][:, :n], B["sq"][:, :n], ALU.mult,
                 reads=[k("t"), k("sq")], writes=[k("t")])
            P.op("dve", "tensor_scalar", B["t"][:, :n], B["t"][:, :n], pc[:, 0:1], pc[:, 1:2], ALU.mult, ALU.add,
                 reads=[k("t"), "pc"], writes=[k("t")])
            P.op("dve", "tensor_tensor", B["t"][:, :n], B["t"][:, :n], B["bo"][:, :n], ALU.add,
                 reads=[k("t"), k("bo")], writes=[k("t")])
            P.op("dve", "tensor_tensor", B["t"][:, :n], B["t"][:, :n], B["g"][:, :n], ALU.mult,
                 reads=[k("t"), k("g")], writes=[k("t")])
            P.dma("sp", o_d[:, t0:t0 + n], B["t"][:, :n], reads=[k("t")], writes=[("out", i)])
        P.finish([("out", i) for i in range(nbuf)])
    return nc, P


def host_readout(streams, ys, inp, l):
    nc, P = cached("readout", build_readout)
    obd = np.zeros((128, 128), np.float32)
    obd[:64, :64] = 1.0 / 64
    obd[64:, 64:] = 1.0 / 64
    maps = []
    for h in range(8):
        hs = slice(h * 64, (h + 1) * 64)
        pc = np.stack([np.concatenate([inp["gn_w"][l][hs]] * 2), np.concatenate([inp["gn_b"][l][hs]] * 2)], axis=1)
        maps.append({"yf": ys[h][0], "yb": ys[h][1], "bonus": streams[h]["bonus"], "g": streams[h]["g"],
                     "pc": np.ascontiguousarray(pc.astype(np.float32)), "obd64": obd})
    res = run_prog(nc, maps)
    o = np.empty((2, TSEQ, 512), np.float32)
    for h in range(8):
        o[:, :, h * 64:(h + 1) * 64] = res[h]["o"].reshape(2, 64, TSEQ).transpose(0, 2, 1)
    return o[:, 256:], o[:, :256]


def fourier_tables():
    k64 = np.arange(64)
    a64 = 2 * np.pi * np.outer(k64, k64) / 64.0
    C64, S64 = np.cos(a64), np.sin(a64)
    t64 = np.stack([np.concatenate([C64, -S64], 1), np.concatenate([S64, C64], 1),
                    np.concatenate([-S64, -C64], 1), np.concatenate([C64, -S64], 1)]).astype(np.float32)
    s1 = np.arange(64)
    t0 = np.arange(128)
    atw = 2 * np.pi * np.outer(s1, t0) / 8192.0
    Tc = np.concatenate([np.cos(atw)] * 2, 0).astype(np.float32)
    Ts = np.concatenate([np.sin(atw)] * 2, 0).astype(np.float32)
    k128 = np.arange(128)
    a128 = 2 * np.pi * np.outer(k128, k128) / 128.0
    sc = 1.0 / np.sqrt(8192.0 * 64.0)
    cs128 = np.stack([np.cos(a128) * sc, np.sin(a128) * sc]).astype(np.float32)
    k256 = np.arange(256)
    a256 = 2 * np.pi * np.outer(k256, k256) / 256.0
    sc2 = 1.0 / np.sqrt(256.0 * 64.0)
    cs256 = np.stack([np.cos(a256) * sc2, np.sin(a256) * sc2]).astype(np.float32).reshape(2, 2, 128, 256)
    return {"t64": t64, "Tc": Tc, "Ts": Ts, "cs128": cs128, "cs256": cs256, "ident": np.eye(128, dtype=np.float32)}


def build_fourier():
    nc, stack, P = new_prog()
    with stack:
        x_d = dram_in(nc, "xT", [64, 8192])
        xc_d = dram_in(nc, "xcT", [64, 256])
        t64_d = dram_in(nc, "t64", [4, 64, 128])
        tc_d = dram_in(nc, "Tc", [128, 128])
        ts_d = dram_in(nc, "Ts", [128, 128])
        cs128_d = dram_in(nc, "cs128", [2, 128, 128])
        cs256_d = dram_in(nc, "cs256", [2, 2, 128, 256])
        id_d = dram_in(nc, "ident", [128, 128])
        y_d = dram_out(nc, "y", [128, 4096])
        yc_d = dram_out(nc, "yc", [2, 128, 64])

        xT = sb(P, "xT_sb", [64, 8192])
        xc = sb(P, "xc_sb", [64, 256])
        t64 = sb(P, "t64_sb", [64, 4, 128])
        Tc = sb(P, "Tc_sb", [128, 128])
        Ts = sb(P, "Ts_sb", [128, 128])
        cs128 = sb(P, "cs128_sb", [128, 2, 128])
        cs256 = sb(P, "cs256_sb", [128, 2, 2, 256])
        ident = sb(P, "ident_sb", [128, 128])
        Zt = sb(P, "Zt", [64, 128, 128])
        Gp = sb(P, "Gp", [128, 128, 64])
        Gt = sb(P, "Gt", [128, 64, 128])
        tmpa = sb(P, "tmpa", [128, 512])
        tmpb = sb(P, "tmpb", [128, 512])
        pp = [ps(P, f"pp{i}", [128, 512]) for i in range(6)]
        P.dma("sp", xT[:], x_d, writes=["xT"])
        P.dma("pool", xc[:], xc_d, writes=["xc"])
        for i in range(4):
            P.dma("sp", t64[:, i, :], t64_d[i], writes=[("t64", i)])
        P.dma("sp", Tc[:], tc_d, writes=["Tc"])
        P.dma("sp", Ts[:], ts_d, writes=["Ts"])
        for i in range(2):
            P.dma("pool", cs128[:, i, :], cs128_d[i], writes=[("cs128", i)])
            for j in range(2):
                P.dma("pool", cs256[:, i, j, :], cs256_d[i, j], writes=[("cs256", i, j)])
        P.dma("sp", ident[:], id_d, writes=["ident"])

        xv = xT[:].rearrange("c (t1 t0) -> c t0 t1", t0=128)
        for q in range(32):
            pt = pp[q % 2]
            pk = ("pp", q % 2)
            for i in range(4):
                t0 = q * 4 + i
                P.op("pe", "matmul", pt[0:64, i * 128:(i + 1) * 128], xv[:, t0, :], t64[:, 0, :], start=True, stop=True,
                     reads=["xT", ("t64", 0)], writes=[pk])
            eng = "act" if q % 2 == 0 else "dve"
            dst = Zt[:, q * 4:(q + 1) * 4, :].rearrange("p a b -> p (a b)")
            if eng == "act":
                P.op("act", "copy", dst, pt[0:64, :], reads=[pk], writes=[("Zt", q // 2)])
            else:
                P.op("dve", "tensor_copy", dst, pt[0:64, :], reads=[pk], writes=[("Zt", q // 2)])
        for q in range(16):
            p1 = pp[2 + (q % 2) * 2]
            p2 = pp[3 + (q % 2) * 2]
            k1 = ("pp", 2 + (q % 2) * 2)
            k2 = ("pp", 3 + (q % 2) * 2)
            zre = Zt[:, q * 8:(q + 1) * 8, 0:64]
            zim = Zt[:, q * 8:(q + 1) * 8, 64:128]
            P.op("pe", "matmul", p1[:].rearrange("p (a b) -> p a b", b=64), t64[:, 0, :], zre, start=True, stop=False,
                 reads=[("t64", 0), ("Zt", q)], writes=[k1])
            P.op("pe", "matmul", p1[:].rearrange("p (a b) -> p a b", b=64), t64[:, 1, :], zim, start=False, stop=True,
                 reads=[("t64", 1), ("Zt", q)], writes=[k1])
            P.op("pe", "matmul", p2[:].rearrange("p (a b) -> p a b", b=64), t64[:, 2, :], zre, start=True, stop=False,
                 reads=[("t64", 2), ("Zt", q)], writes=[k2])
            P.op("pe", "matmul", p2[:].rearrange("p (a b) -> p a b", b=64), t64[:, 3, :], zim, start=False, stop=True,
                 reads=[("t64", 3), ("Zt", q)], writes=[k2])
            tcb = Tc[:, q * 8:(q + 1) * 8].unsqueeze(2).broadcast_to([128, 8, 64])
            tsb = Ts[:, q * 8:(q + 1) * 8].unsqueeze(2).broadcast_to([128, 8, 64])
            P.op("dve", "tensor_tensor", tmpa[:].rearrange("p (a b) -> p a b", b=64),
                 p1[:].rearrange("p (a b) -> p a b", b=64), tcb, ALU.mult, reads=[k1, "Tc"], writes=["tmpa"])
            P.op("dve", "tensor_tensor", tmpb[:].rearrange("p (a b) -> p a b", b=64),
                 p2[:].rearrange("p (a b) -> p a b", b=64), tsb, ALU.mult, reads=[k2, "Ts"], writes=["tmpb"])
            P.op("dve", "tensor_tensor", Gp[:, q * 8:(q + 1) * 8, :].rearrange("p a b -> p (a b)"), tmpa[:], tmpb[:], ALU.add,
                 reads=["tmpa", "tmpb"], writes=[("Gp", q)])
        for q in range(16):
            pt = pp[q % 2]
            pk = ("pp", q % 2)
            for i in range(4):
                c = q * 4 + i
                P.op("pe", "transpose", pt[:, i * 128:(i + 1) * 128], Gp[:, :, c], ident[:],
                     reads=[("Gp", j) for j in range(16)] + ["ident"], writes=[pk])
            dst = Gt[:, q * 4:(q + 1) * 4, :].rearrange("p a b -> p (a b)")
            if q % 2 == 0:
                P.op("act", "copy", dst, pt[:], reads=[pk], writes=[("Gt", q // 2)])
            else:
                P.op("dve", "tensor_copy", dst, pt[:], reads=[pk], writes=[("Gt", q // 2)])
        Y = Zt
        Yv = Gp[:, 0:64, :].rearrange("p s c -> p c s")
        for q in range(8):
            pt = pp[2 + (q % 2) * 2]
            pk = ("pp", 2 + (q % 2) * 2)
            P.op("pe", "matmul", pt[:].rearrange("p (a b) -> p a b", b=64), cs128[:, 0, :], Gt[:, q * 8:(q + 1) * 8, 0:64],
                 start=True, stop=False, reads=[("cs128", 0), ("Gt", q)], writes=[pk])
            P.op("pe", "matmul", pt[:].rearrange("p (a b) -> p a b", b=64), cs128[:, 1, :], Gt[:, q * 8:(q + 1) * 8, 64:128],
                 start=False, stop=True, reads=[("cs128", 1), ("Gt", q)], writes=[pk])
            P.op("dve", "tensor_copy", Yv[:, q * 8:(q + 1) * 8, :], pt[:].rearrange("p (a b) -> p a b", b=64),
                 reads=[pk] + [("Gt", j) for j in range(8)], writes=[("Gp", j) for j in range(16)])
        P.dma("sp", y_d, Gp[:, 0:64, :].rearrange("p a b -> p (a b)"), reads=[("Gp", j) for j in range(16)], writes=["yout"])
        AB = sb(P, "AB", [128, 2, 128])
        yc = sb(P, "yc_sb", [128, 2, 64])
        for m in range(2):
            pt = pp[m]
            P.op("pe", "matmul", pt[:, 0:128], xc[:, m * 128:(m + 1) * 128], t64[:, 0, :], start=True, stop=True,
                 reads=["xc", ("t64", 0)], writes=[("pp", m)])
            P.op("act", "copy", AB[:, m, :], pt[:, 0:128], reads=[("pp", m)], writes=[("AB", m)])
        for sc_ in range(2):
            pt = pp[2 + sc_]
            first = True
            for m in range(2):
                for i in range(2):
                    P.op("pe", "matmul", pt[:, 0:64], cs256[:, i, m, sc_ * 128:(sc_ + 1) * 128], AB[:, m, i * 64:(i + 1) * 64],
                         start=first, stop=(m == 1 and i == 1),
                         reads=[("cs256", i, m), ("AB", m)], writes=[("pp", 2 + sc_)])
                    first = False
            P.op("dve", "tensor_copy", yc[:, sc_, :], pt[:, 0:64], reads=[("pp", 2 + sc_)], writes=[("yc", sc_)])
            P.dma("sp", yc_d[sc_], yc[:, sc_, :], reads=[("yc", sc_)], writes=[("ycout", sc_)])
        P.finish(["yout", ("ycout", 0), ("ycout", 1)])
    return nc, P


def host_fourier(plat, pctx):
    nc, P = cached("fourier", build_fourier)
    tabs = fourier_tables()
    maps = []
    for i in range(8):
        b, g = i // 4, i % 4
        cs = slice(2688 + g * 64, 2688 + (g + 1) * 64)
        m = dict(tabs)
        m["xT"] = np.ascontiguousarray(plat[b, :, cs].T)
        m["xcT"] = np.ascontiguousarray(pctx[b, :, cs].T)
        maps.append(m)
    res = run_prog(nc, maps)
    lat = np.empty((2, 8192, 256), np.float32)
    ctx = np.empty((2, 256, 256), np.float32)
    for i in range(8):
        b, g = i // 4, i % 4
        lat[b, :, g * 64:(g + 1) * 64] = res[i]["y"].reshape(8192, 64)
        ctx[b, :, g * 64:(g + 1) * 64] = res[i]["yc"].reshape(256, 64)
    return lat, ctx


NHAL = 2116
BIG = 1.0e30


def build_g1():
    nc, stack, P = new_prog()
    with stack:
        x_d = dram_in(nc, "xT", [8, 128, NTOK])
        o_d = dram_in(nc, "oT", [4, 128, NTOK])
        cv_d = dram_in(nc, "cvT", [6, 128, NHAL])
        f_d = dram_in(nc, "fT", [2, 128, NTOK])
        wo_d = dram_in(nc, "wout", [8, 128, 1024])
        cols_d = dram_in(nc, "cols", [128, 2, 4, 8])
        cvp_d = dram_in(nc, "cvp", [128, 2, 4])
        fg_d = dram_in(nc, "fg", [128, 2])
        rw_d = dram_in(nc, "rw", [8, 128, 36])
        rb_d = dram_in(nc, "rb", [36, 1])
        id_d = dram_in(nc, "ident", [128, 128])
        xo_d = dram_out(nc, "xo", [8, 128, NTOK])
        h2_d = dram_out(nc, "h2", [8, 128, NTOK])
        gm_d = dram_out(nc, "gm", [NTOK, 32])

        xT = sb(P, "xT_sb", [128, 8, NTOK])
        catb = sb(P, "catb", [128, 8, NTOK], BF16)
        wo = sb(P, "wo_sb", [128, 8, 1024], BF16)
        cols = sb(P, "cols_sb", [128, 2, 4, 8])
        geff = sb(P, "geff", [128, 2, 8])
        cvp = sb(P, "cvp_sb", [128, 2, 4])
        fg = sb(P, "fg_sb", [128, 2])
        rw = sb(P, "rw_sb", [128, 8, 36])
        rb = sb(P, "rb_sb", [36, 1])
        ident = sb(P, "ident_sb", [128, 128])
        ones = sb(P, "ones", [128, 128])
        sq = sb(P, "sq", [128, 512])
        tmp = sb(P, "tmp", [128, 512])
        rstd = sb(P, "rstd", [128, 512])
        h2f = sb(P, "h2f", [128, 8, 512])
        cvin = sb(P, "cvin", [128, 6, 514])
        zc = sb(P, "zc", [128, 514])
        cacc = sb(P, "cacc", [128, 2, 512])
        fin = sb(P, "fin", [128, 2, 512])
        LT = sb(P, "LT", [36, 512])
        pss = ps(P, "pss", [128, 512])
        pso = [ps(P, f"pso{i}", [128, 512]) for i in range(2)]
        plg = ps(P, "plg", [36, 512])
        ptr = ps(P, "ptr", [128, 36])
        P.op("dve", "memset", ones[:], 1.0, writes=["ones"])
        P.dma("sp", cols[:], cols_d, writes=["cols"])
        P.dma("sp", cvp[:], cvp_d, writes=["cvp"])
        P.dma("sp", fg[:], fg_d, writes=["fg"])
        P.dma("sp", rb[:], rb_d, writes=["rb"])
        P.dma("sp", ident[:], id_d, writes=["ident"])
        for k in range(8):
            P.dma("sp", xT[:, k, :], x_d[k], writes=[("G", "x", k)])
            P.dma("pool", wo[:, k, :], wo_d[k], writes=[("wo", k)])
            P.dma("sp", rw[:, k, :], rw_d[k], writes=[("rw", k)])
        for k in range(4):
            P.dma("pool", catb[:, k, :], o_d[k], writes=[("cat", k)])
        for s in range(2):
            P.op("dve", "scalar_tensor_tensor", geff[:, s, :], cols[:, s, 2, :], 1.0, cols[:, s, 1, :],
                 ALU.add, ALU.mult, reads=["cols"], writes=[("G", "cols")])

        def branch_norm(src_tiles, srck, gain_cols, kbase, c0, n):
            for j in range(2):
                P.op("act", "activation", sq[:, :n], src_tiles[j], AF.Square, reads=[srck], writes=["sqb"])
                P.op("pe", "matmul", pss[:, :n], ones[:], sq[:, :n], start=(j == 0), stop=(j == 1),
                     reads=["sqb", "ones"], writes=["pss"])
            P.op("dve", "tensor_scalar", rstd[:, :n], pss[:, :n], 1.0 / 256.0, RMS_EPS, ALU.mult, ALU.add,
                 reads=["pss"], writes=["rstdb"])
            P.op("act", "activation", rstd[:, :n], rstd[:, :n], AF.Sqrt, reads=["rstdb"], writes=["rstdb"])
            P.op("dve", "reciprocal", rstd[:, :n], rstd[:, :n], reads=["rstdb"], writes=["rstdb"])
            for j in range(2):
                P.op("dve", "scalar_tensor_tensor", catb[:, kbase + j, c0:c0 + n], src_tiles[j], gain_cols[j], rstd[:, :n],
                     ALU.mult, ALU.mult, reads=[srck, "rstdb", "cvp", "fg"], writes=[("cat", kbase + j)])

        tile_idx = 0
        for (c0, n, seg) in CHUNKS:
            hc0 = c0 if seg == 0 else 2050
            for j in range(6):
                w = n if j < 2 else n + 2
                off = hc0 + (1 if j < 2 else 0)
                P.dma("sp" if j % 2 == 0 else "pool", cvin[:, j, :w], cv_d[j, :, off:off + w], writes=[("cvin", j)])
            for j in range(2):
                P.op("dve", "tensor_tensor", zc[:, :n + 2], cvin[:, 2 + j, :n + 2], cvin[:, 4 + j, :n + 2], ALU.mult,
                     reads=[("cvin", 2 + j), ("cvin", 4 + j)], writes=["zc"])
                P.op("dve", "tensor_scalar", cacc[:, j, :n], zc[:, 1:n + 1], cvp[:, j, 1:2], None, ALU.mult,
                     reads=["zc", "cvp"], writes=["cacc"])
                P.op("dve", "scalar_tensor_tensor", cacc[:, j, :n], zc[:, 0:n], cvp[:, j, 0:1], cacc[:, j, :n],
                     ALU.mult, ALU.add, reads=["zc", "cvp", "cacc"], writes=["cacc"])
                P.op("dve", "scalar_tensor_tensor", cacc[:, j, :n], zc[:, 2:n + 2], cvp[:, j, 2:3], cacc[:, j, :n],
                     ALU.mult, ALU.add, reads=["zc", "cvp", "cacc"], writes=["cacc"])
                P.op("dve", "tensor_tensor", cacc[:, j, :n], cacc[:, j, :n], cvin[:, j, :n], ALU.mult,
                     reads=["cacc", ("cvin", j)], writes=["cacc"])
            branch_norm([cacc[:, 0, :n], cacc[:, 1, :n]], "cacc", [cvp[:, 0, 3:4], cvp[:, 1, 3:4]], 4, c0, n)
            for j in range(2):
                P.dma("sp", fin[:, j, :n], f_d[j, :, c0:c0 + n], writes=["fin"], sem=("fin", j))
            branch_norm([fin[:, 0, :n], fin[:, 1, :n]], "fin", [fg[:, 0:1], fg[:, 1:2]], 6, c0, n)
            for j in range(8):
                pt = pso[j % 2]
                for k in range(8):
                    P.op("pe", "matmul", pt[:, :n], wo[:, k, j * 128:(j + 1) * 128], catb[:, k, c0:c0 + n],
                         start=(k == 0), stop=(k == 7), reads=[("wo", k), ("cat", k)], writes=[("pso", j % 2)])
                P.op("dve", "scalar_tensor_tensor", xT[:, j, c0:c0 + n], pt[:, :n], cols[:, seg, 0, j:j + 1], xT[:, j, c0:c0 + n],
                     ALU.mult, ALU.add, reads=[("pso", j % 2), "cols", ("G", "x", j)], writes=[("G", "x", j)])
                P.dma("sp", xo_d[j, :, c0:c0 + n], xT[:, j, c0:c0 + n], reads=[("G", "x", j)], writes=[("xo", j)])
            emit_rmsnorm_mod(P, "G", xT, c0, n, geff[:, seg, :], cols[:, seg, 3, :], h2f, ones, sq, pss, rstd, tmp,
                             h0=0, hkey="h2f")
            for k in range(8):
                P.dma("pool", h2_d[k, :, c0:c0 + n], h2f[:, k, :n], reads=["h2f"], writes=[("h2o", k)])
            for k in range(8):
                P.op("pe", "matmul", plg[:, :n], rw[:, k, :], h2f[:, k, :n], start=(k == 0), stop=(k == 7),
                     reads=[("rw", k), "h2f"], writes=["plg"])
            P.op("act", "activation", LT[:, :n], plg[:, :n], AF.Identity, bias=rb[:, 0:1], reads=["plg", "rb"], writes=["LT"])
            for j in range(0, n, 128):
                m = min(128, n - j)
                ti = tile_idx
                tile_idx += 1
                L = sb(P, f"L{ti}", [128, 36])
                W = sb(P, f"W{ti}", [128, 80])
                G = sb(P, f"Gm{ti}", [128, 32])
                lk, wk, gk = ("L", ti), ("W", ti), ("Gm", ti)
                P.op("pe", "transpose", ptr[:m, :], LT[:, j:j + m], ident[:36, :36], reads=["LT", "ident"], writes=["ptr"])
                P.op("dve", "tensor_copy", L[:m, :], ptr[:m, :], reads=["ptr"], writes=[lk])
                gmax, ngmax, seg_, pgt = W[:m, 0:1], W[:m, 1:2], W[:m, 2:3], W[:m, 3:4]
                ohg, eg = W[:m, 4:8], W[:m, 8:12]
                m1, m2, d12, s1, w1, w2 = W[:m, 12:13], W[:m, 13:14], W[:m, 14:15], W[:m, 15:16], W[:m, 16:17], W[:m, 17:18]
                lem = W[:m, 18:50]
                P.op("dve", "tensor_reduce", gmax, L[:m, 0:4], AX.X, ALU.max, reads=[lk], writes=[wk])
                P.op("dve", "tensor_scalar", ngmax, gmax, -1.0, None, ALU.mult, reads=[wk], writes=[wk])
                P.op("act", "activation", eg, L[:m, 0:4], AF.Exp, bias=ngmax, reads=[lk, wk], writes=[wk])
                P.op("dve", "tensor_reduce", seg_, eg, AX.X, ALU.add, reads=[wk], writes=[wk])
                P.op("dve", "reciprocal", pgt, seg_, reads=[wk], writes=[wk])
                P.op("dve", "tensor_scalar", ohg, L[:m, 0:4], gmax, None, ALU.is_equal, reads=[lk, wk], writes=[wk])
                P.op("dve", "tensor_scalar", ohg, ohg, -1.0, BIG, ALU.add, ALU.mult, reads=[wk], writes=[wk])
                P.op("dve", "tensor_tensor", lem.rearrange("p (g e) -> p g e", e=8), L[:m, 4:36].rearrange("p (g e) -> p g e", e=8),
                     ohg.unsqueeze(2).broadcast_to([m, 4, 8]), ALU.add, reads=[lk, wk], writes=[wk])
                P.op("dve", "tensor_reduce", m1, lem, AX.X, ALU.max, reads=[wk], writes=[wk])
                oh1 = W[:m, 50:82] if False else None
                O1 = sb(P, f"O1_{ti}", [128, 32])
                O2 = sb(P, f"O2_{ti}", [128, 32])
                o1k, o2k = ("O1", ti), ("O2", ti)
                P.op("dve", "tensor_scalar", O1[:m, :], lem, m1, None, ALU.is_equal, reads=[wk], writes=[o1k])
                P.op("dve", "scalar_tensor_tensor", lem, O1[:m, :], -BIG, lem, ALU.mult, ALU.add, reads=[o1k, wk], writes=[wk])
                P.op("dve", "tensor_reduce", m2, lem, AX.X, ALU.max, reads=[wk], writes=[wk])
                P.op("dve", "tensor_scalar", O2[:m, :], lem, m2, None, ALU.is_equal, reads=[wk], writes=[o2k])
                P.op("dve", "tensor_tensor", d12, m1, m2, ALU.subtract, reads=[wk], writes=[wk])
                P.op("act", "activation", s1, d12, AF.Sigmoid, reads=[wk], writes=[wk])
                P.op("dve", "tensor_tensor", w1, s1, pgt, ALU.mult, reads=[wk], writes=[wk])
                P.op("dve", "tensor_tensor", w2, pgt, w1, ALU.subtract, reads=[wk], writes=[wk])
                P.op("dve", "tensor_scalar", G[:m, :], O1[:m, :], w1, None, ALU.mult, reads=[o1k, wk], writes=[gk])
                P.op("dve", "scalar_tensor_tensor", G[:m, :], O2[:m, :], w2, G[:m, :], ALU.mult, ALU.add,
                     reads=[o2k, wk, gk], writes=[gk])
                P.dma("sp", gm_d[c0 + j:c0 + j + m, :], G[:m, :], reads=[gk], writes=[("gmo", ti)])
        P.finish([("xo", j) for j in range(8)] + [("h2o", k) for k in range(8)] + [("gmo", t) for t in range(tile_idx)])
    return nc, P


def halo_cols(lat, ctx, i):
    b, q = i // 4, i % 4
    C = lat.shape[2]
    out = np.zeros((C, NHAL), np.float32)
    lo, hi = q * 2048 - 1, (q + 1) * 2048 + 1
    a, e = max(lo, 0), min(hi, 8192)
    out[:, (a - lo):(a - lo) + (e - a)] = lat[b, a:e].T
    lo, hi = q * 64 - 1, (q + 1) * 64 + 1
    a, e = max(lo, 0), min(hi, 256)
    out[:, 2050 + (a - lo):2050 + (a - lo) + (e - a)] = ctx[b, a:e].T
    return out


def host_g1(xlat, xctx, olat, octx, plat, pctx, flat, fctx, mod_l, inp, l):
    nc, P = cached("g1", build_g1)
    xs = shard_tokens_T(xlat, xctx)
    os_ = shard_tokens_T(olat, octx)
    fs = shard_tokens_T(flat, fctx)
    wo = np.ascontiguousarray(inp["w_out"][l].reshape(8, 128, 1024))
    rwf = np.concatenate([inp["router_g_w"][l], inp["router_e_w"][l]], axis=1)
    rw = np.ascontiguousarray(rwf.reshape(8, 128, 36))
    rb = np.concatenate([inp["router_g_b"][l], inp["router_e_b"][l]])[:, None].astype(np.float32)
    cvp = np.zeros((128, 2, 4), np.float32)
    for j in range(2):
        for t in range(3):
            cvp[:, j, t] = inp["conv_w"][l][t, j * 128:(j + 1) * 128]
        cvp[:, j, 3] = inp["conv_gain"][l][j * 128:(j + 1) * 128]
    fg = np.ascontiguousarray(inp["four_gain"][l].reshape(2, 128).T)
    ident = np.eye(128, dtype=np.float32)
    maps = []
    for i in range(8):
        b = i // 4
        cols = np.zeros((128, 2, 4, 8), np.float32)
        for s, row in enumerate((b, 2)):
            cols[:, s, 0, :] = col_layout(mod_l[row, 2048:3072])
            cols[:, s, 1, :] = col_layout(inp["norm2_g"][l])
            cols[:, s, 2, :] = col_layout(mod_l[row, 4096:5120])
            cols[:, s, 3, :] = col_layout(mod_l[row, 3072:4096])
        cv = halo_cols(plat[:, :, 1920:2688], pctx[:, :, 1920:2688], i)
        maps.append({"xT": xs[i].reshape(8, 128, NTOK), "oT": os_[i].reshape(4, 128, NTOK),
                     "cvT": cv.reshape(6, 128, NHAL), "fT": fs[i].reshape(2, 128, NTOK), "wout": wo,
                     "cols": cols, "cvp": cvp, "fg": fg, "rw": rw, "rb": rb, "ident": ident})
    res = run_prog(nc, maps)
    xo = unshard_tokens_T([r["xo"].reshape(1024, NTOK) for r in res])
    h2 = [r["h2"] for r in res]
    gm = [r["gm"] for r in res]
    return xo, h2, gm


FCHUNKS = [(c0, min(256, NTOK - c0)) for c0 in range(0, NTOK, 256)]


def build_g2():
    nc, stack, P = new_prog()
    with stack:
        x_d = dram_in(nc, "xT", [8, 128, NTOK])
        h_d = dram_in(nc, "h2", [8, 128, NTOK])
        gt_d = dram_in(nc, "GT", [32, NTOK])
        wg_d = dram_in(nc, "wg", [32, 8, 128, 512])
        wu_d = dram_in(nc, "wu", [32, 8, 128, 512])
        wd_d = dram_in(nc, "wd", [32, 4, 128, 1024])
        cols_d = dram_in(nc, "cols", [128, 2, 8])
        fgn_d = dram_in(nc, "fgn", [128, 8])
        e32_d = dram_in(nc, "e32", [32, 32])
        xo_d = dram_out(nc, "xo", [8, 128, NTOK])
        xf_d = dram_out(nc, "xf", [8, 128, NTOK])

        xT = sb(P, "xT_sb", [128, 8, NTOK])
        h2b = sb(P, "h2b", [128, 8, NTOK], BF16)
        GT = sb(P, "GT_sb", [32, NTOK])
        wbuf = [sb(P, f"wbuf{i}", [128, 12288], BF16) for i in range(2)]
        cols = sb(P, "cols_sb", [128, 2, 8])
        fgn = sb(P, "fgn_sb", [128, 8])
        zero8 = sb(P, "zero8", [128, 8])
        e32 = sb(P, "e32_sb", [32, 32])
        ones = sb(P, "ones", [128, 128])
        sl = sb(P, "sl", [128, 512])
        hid = sb(P, "hid", [128, 512])
        hidg = [[sb(P, f"hidg{i}_{f}", [128, 512], BF16) for f in range(4)] for i in range(2)]
        sq = sb(P, "sq", [128, 256])
        tmp = sb(P, "tmp", [128, 256])
        rstd = sb(P, "rstd", [128, 256])
        hfin = sb(P, "hfin", [128, 8, 256])
        psg = [ps(P, f"psg{i}", [128, 512]) for i in range(2)]
        psu = [ps(P, f"psu{i}", [128, 512]) for i in range(2)]
        pgb = ps(P, "pgb", [128, 512])
        pso = [ps(P, f"pso{i}", [128, 512]) for i in range(2)]
        pss = ps(P, "pss", [128, 256])
        P.op("dve", "memset", ones[:], 1.0, writes=["ones"])
        P.op("dve", "memset", zero8[:], 0.0, writes=["zero8"])
        P.dma("sp", cols[:], cols_d, writes=["cols"])
        P.dma("sp", fgn[:], fgn_d, writes=["fgn"])
        P.dma("sp", e32[:], e32_d, writes=["e32"])
        P.dma("sp", GT[:], gt_d, writes=["GT"])
        for k in range(8):
            P.dma("sp", xT[:, k, :], x_d[k], writes=[("F", "x", k)])
            P.dma("pool", h2b[:, k, :], h_d[k], writes=[("h2b", k)])

        def load_w(e):
            b = e % 2
            wb = wbuf[b]
            P.dma("pool", wb[:, 0:4096].rearrange("p (k f) -> p k f", f=512), wg_d[e].rearrange("k p f -> p k f"),
                  writes=[("wg", b)])
            P.dma("pool", wb[:, 4096:8192].rearrange("p (k f) -> p k f", f=512), wu_d[e].rearrange("k p f -> p k f"),
                  writes=[("wu", b)])
            P.dma("pool", wb[:, 8192:12288].rearrange("p (k f) -> p k f", f=1024), wd_d[e].rearrange("k p f -> p k f"),
                  writes=[("wd", b)])

        load_w(0)
        cnt = 0
        hcnt = 0
        for e in range(32):
            if e + 1 < 32:
                load_w(e + 1)
            b = e % 2
            Wg = wbuf[b][:, 0:4096].rearrange("p (k f) -> p k f", f=512)
            Wu = wbuf[b][:, 4096:8192].rearrange("p (k f) -> p k f", f=512)
            Wd = wbuf[b][:, 8192:12288].rearrange("p (k f) -> p k f", f=1024)
            for (c0, n, seg) in CHUNKS:
                P.op("pe", "matmul", pgb[:, :n], e32[:, e:e + 1].broadcast_to([32, 128]), GT[:, c0:c0 + n], start=True, stop=True,
                     reads=["e32", "GT"], writes=["pgb"])
                hs = hcnt % 2
                hcnt += 1
                for f in range(4):
                    i = cnt % 2
                    cnt += 1
                    for k in range(8):
                        P.op("pe", "matmul", psg[i][:, :n], Wg[:, k, f * 128:(f + 1) * 128], h2b[:, k, c0:c0 + n],
                             start=(k == 0), stop=(k == 7), reads=[("wg", b), ("h2b", k)], writes=[("psg", i)])
                    for k in range(8):
                        P.op("pe", "matmul", psu[i][:, :n], Wu[:, k, f * 128:(f + 1) * 128], h2b[:, k, c0:c0 + n],
                             start=(k == 0), stop=(k == 7), reads=[("wu", b), ("h2b", k)], writes=[("psu", i)])
                    P.op("act", "activation", sl[:, :n], psg[i][:, :n], AF.Silu, reads=[("psg", i)], writes=["sl"])
                    P.op("dve", "tensor_tensor", hid[:, :n], sl[:, :n], psu[i][:, :n], ALU.mult,
                         reads=["sl", ("psu", i)], writes=["hid"])
                    P.op("dve", "tensor_tensor", hidg[hs][f][:, :n], hid[:, :n], pgb[:, :n], ALU.mult,
                         reads=["hid", "pgb"], writes=[("hidg", hs, f)])
                for j in range(8):
                    pt = pso[j % 2]
                    for f in range(4):
                        P.op("pe", "matmul", pt[:, :n], Wd[:, f, j * 128:(j + 1) * 128], hidg[hs][f][:, :n],
                             start=(f == 0), stop=(f == 3), reads=[("wd", b), ("hidg", hs, f)], writes=[("pso", j % 2)])
                    P.op("dve", "scalar_tensor_tensor", xT[:, j, c0:c0 + n], pt[:, :n], cols[:, seg, j:j + 1], xT[:, j, c0:c0 + n],
                         ALU.mult, ALU.add, reads=[("pso", j % 2), "cols", ("F", "x", j)], writes=[("F", "x", j)])
        for k in range(8):
            P.dma("sp", xo_d[k], xT[:, k, :], reads=[("F", "x", k)], writes=[("xo", k)])
        for (c0, n) in FCHUNKS:
            emit_rmsnorm_mod(P, "F", xT, c0, n, fgn, zero8, hfin, ones, sq, pss, rstd, tmp, h0=0, hkey="hfin")
            for k in range(8):
                P.dma("sp" if k % 2 == 0 else "pool", xf_d[k, :, c0:c0 + n], hfin[:, k, :n], reads=["hfin"], writes=[("xf", k)])
        P.finish([("xo", k) for k in range(8)] + [("xf", k) for k in range(8)])
    return nc, P


def host_g2(xlat, xctx, h2, gm, mod_l, inp, l):
    nc, P = cached("g2", build_g2)
    xs = shard_tokens_T(xlat, xctx)
    wg = np.ascontiguousarray(inp["exp_gate"][l].reshape(32, 8, 128, 512))
    wu = np.ascontiguousarray(inp["exp_up"][l].reshape(32, 8, 128, 512))
    wd = np.ascontiguousarray(inp["exp_down"][l].reshape(32, 4, 128, 1024))
    fgn = col_layout(inp["final_g"])
    e32 = np.eye(32, dtype=np.float32)
    maps = []
    for i in range(8):
        b = i // 4
        cols = np.zeros((128, 2, 8), np.float32)
        cols[:, 0, :] = col_layout(mod_l[b, 5120:6144])
        cols[:, 1, :] = col_layout(mod_l[2, 5120:6144])
        maps.append({"xT": xs[i].reshape(8, 128, NTOK), "h2": h2[i], "GT": np.ascontiguousarray(gm[i].T),
                     "wg": wg, "wu": wu, "wd": wd, "cols": cols, "fgn": fgn, "e32": e32})
    res = run_prog(nc, maps)
    xo = unshard_tokens_T([r["xo"].reshape(1024, NTOK) for r in res])
    xf = unshard_tokens_T([r["xf"].reshape(1024, NTOK) for r in res])
    return xo, xf


def kernel(**inputs):
    inp = {k: np.asarray(v, dtype=np.float32) for k, v in inputs.items()}
    mod = host_mods(inp)
    xlat, xctx = inp["x"], inp["ctx"]
    xf = None
    for l in range(2):
        plat, pctx = host_proj(xlat, xctx, mod[l], inp["norm1_g"][l], inp["w_in"][l])
        streams = host_streams(plat, pctx, inp, l)
        ys = host_scan(streams)
        olat, octx = host_readout(streams, ys, inp, l)
        flat, fctx = host_fourier(plat, pctx)
        (xlat, xctx), h2, gm = host_g1(xlat, xctx, olat, octx, plat, pctx, flat, fctx, mod[l], inp, l)
        (xlat, xctx), xf = host_g2(xlat, xctx, h2, gm, mod[l], inp, l)
    return np.ascontiguousarray(xf[0].astype(np.float32))
```

```python
import contextlib
import numpy as np
import concourse.bass as bass
import concourse.mybir as mybir
from concourse.bass_utils import run_bass_kernel_spmd

F32 = mybir.dt.float32
BF16 = mybir.dt.bfloat16
ALU = mybir.AluOpType
AF = mybir.ActivationFunctionType
AX = mybir.AxisListType


class Prog:
    def __init__(self, nc, stack):
        self.nc = nc
        self.stack = stack
        self.engs = {"pe": nc.tensor, "act": nc.scalar, "dve": nc.vector, "pool": nc.gpsimd, "sp": nc.sync}
        self.sem = {}
        self.seq = {}
        for name in ["pe", "act", "dve", "pool"]:
            self.sem[name] = stack.enter_context(nc.semaphore("s_" + name))
            self.seq[name] = 0
        self.known = {e: {} for e in self.engs}
        self.last_write = {}
        self.reads_since = {}
        self.n_instr = 0

    def _wait(self, eng, src, val):
        if val <= 0:
            return
        if self.known[eng].get(src, 0) >= val:
            return
        self.engs[eng].wait_ge(self.sem[src], val)
        self.known[eng][src] = val
        self.n_instr += 1

    def _deps(self, eng, me, reads, writes):
        need = {}

        def add(ev, same_ok):
            if ev is None:
                return
            src, val = ev
            if src == me and same_ok:
                return
            if need.get(src, 0) < val:
                need[src] = val

        for k in reads:
            add(self.last_write.get(k), False)
        for k in writes:
            add(self.last_write.get(k), me == "pe")
            for src, val in self.reads_since.get(k, {}).items():
                add((src, val), False)
        for src, val in need.items():
            self._wait(eng, src, val)

    def _record(self, me, val, reads, writes):
        for k in reads:
            d = self.reads_since.setdefault(k, {})
            if d.get(me, 0) < val:
                d[me] = val
        for k in writes:
            self.last_write[k] = (me, val)
            self.reads_since[k] = {}

    def op(self, eng, fn, *args, reads=(), writes=(), **kw):
        self._deps(eng, eng, reads, writes)
        ins = getattr(self.engs[eng], fn)(*args, **kw)
        ins.then_inc(self.sem[eng], 1)
        self.seq[eng] += 1
        self.n_instr += 1
        self._record(eng, self.seq[eng], reads, writes)
        return ins

    def dma(self, q, out, in_, reads=(), writes=(), sem=None, **kw):
        if sem is None:
            sem = writes[0]
        me = ("dma", sem)
        if me not in self.sem:
            self.sem[me] = self.stack.enter_context(self.nc.semaphore("sd%d" % len(self.sem)))
            self.seq[me] = 0
        self._wait(q, me, self.seq[me])
        self._deps(q, me, reads, writes)
        ins = self.engs[q].dma_start(out=out, in_=in_, **kw)
        ins.then_inc(self.sem[me], 16)
        self.seq[me] += 16
        self.n_instr += 1
        self._record(me, self.seq[me], reads, writes)
        return ins

    def finish(self, keys):
        for k in keys:
            ev = self.last_write.get(k)
            if ev is not None:
                self._wait("sp", ev[0], ev[1])


def new_prog():
    nc = bass.Bass("TRN2", target_bir_lowering=False)
    stack = contextlib.ExitStack()
    P = Prog(nc, stack)
    return nc, stack, P


def sb(P, name, shape, dt=F32):
    return P.stack.enter_context(P.nc.sbuf_tensor(name, list(shape), dt))


def ps(P, name, shape, dt=F32):
    return P.stack.enter_context(P.nc.psum_tensor(name, list(shape), dt))


def dram_in(nc, name, shape, dt=F32):
    return nc.dram_tensor(name, list(shape), dt, kind="ExternalInput").ap()


def dram_out(nc, name, shape, dt=F32):
    return nc.dram_tensor(name, list(shape), dt, kind="ExternalOutput").ap()


SCAN_CH = 8
YCH = 256


def build_scan(T):
    nc, stack, P = new_prog()
    with stack:
        rows_d = [dram_in(nc, f"rows{d}", [2, T, 448]) for d in range(2)]
        dcol_d = [dram_in(nc, f"dcol{d}", [128, T]) for d in range(2)]
        rpad_d = [dram_in(nc, f"rpad{d}", [128, T, 2]) for d in range(2)]
        ident_d = dram_in(nc, "ident", [128, 128])
        y_d = [dram_out(nc, f"y{d}", [64, T, 2]) for d in range(2)]

        ident = sb(P, "ident_sb", [128, 128])
        P.dma("sp", ident[:], ident_d, writes=["ident"])
        NCOL = 1056
        assert T % SCAN_CH == 0
        st = []
        for d in range(2):
            s = dict(
                rows=[sb(P, f"rows_sb{d}_{i}", [2, SCAN_CH, 448]) for i in range(2)],
                dcol=[sb(P, f"dcol_sb{d}_{i}", [128, NCOL]) for i in range(2)],
                rpad=[sb(P, f"rpad_sb{d}_{i}", [128, NCOL, 2]) for i in range(2)],
                H=[sb(P, f"H{d}_{i}", [128, 64]) for i in range(2)],
                Pt=[sb(P, f"Pt{d}_{i}", [128, 128]) for i in range(3)],
                psP=[ps(P, f"psP{d}_{i}", [128, 128])[:] for i in range(2)],
                psH=[ps(P, f"psH{d}", [128, 64])[:]] * 2,
                psY=[ps(P, f"psY{d}", [64, YCH, 2])] * 2,
                ysb=[sb(P, f"ysb{d}_{i}", [64, YCH, 2]) for i in range(2)],
            )
            st.append(s)
            P.op("dve", "memset", s["H"][1][:], 0.0, writes=[("H", d, 1)])

        def load_rows(d, c):
            s = st[d]
            b = c % 2
            q = "sp" if d == 0 else "pool"
            P.dma(q, s["rows"][b][:], rows_d[d][:, c * SCAN_CH:(c + 1) * SCAN_CH, :], writes=[("rows", d, b)])

        def load_cols(d, c):
            s = st[d]
            b = c % 2
            n = min(NCOL, T - c * NCOL)
            q = "sp" if d == 0 else "pool"
            P.dma(q, s["dcol"][b][:, :n], dcol_d[d][:, c * NCOL:c * NCOL + n], writes=[("dcol", d, b)])
            P.dma(q, s["rpad"][b][:, :n, :], rpad_d[d][:, c * NCOL:c * NCOL + n, :], writes=[("rpad", d, b)])

        def mmP(d, t):
            s = st[d]
            rb = (t // SCAN_CH) % 2
            j = t % SCAN_CH
            rows = s["rows"][rb]
            P.op("pe", "matmul", s["psP"][t % 2], rows[:, j, 0:128], rows[:, j, 128:256],
                 start=True, stop=True,
                 reads=[("rows", d, rb)], writes=[("psP", d, t % 2)])

        def step(d, t):
            s = st[d]
            rb = (t // SCAN_CH) % 2
            j = t % SCAN_CH
            cb = (t // NCOL) % 2
            jc = t % NCOL
            rows = s["rows"][rb]
            hp, hn = (t + 1) % 2, t % 2
            P.op("dve", "scalar_tensor_tensor", s["Pt"][t % 3][:], ident[:], s["dcol"][cb][:, jc:jc + 1],
                 s["psP"][t % 2], ALU.mult, ALU.add,
                 reads=["ident", ("dcol", d, cb), ("psP", d, t % 2)], writes=[("Pt", d, t % 3)])
            P.op("pe", "matmul", s["psH"][t % 2], s["Pt"][t % 3][:], s["H"][hp][:], start=True, stop=False,
                 reads=[("Pt", d, t % 3), ("H", d, hp)], writes=[("psH", d)])
            P.op("pe", "matmul", s["psH"][t % 2], rows[:, j, 256:384], rows[:, j, 384:448], start=False, stop=True,
                 reads=[("rows", d, rb)], writes=[("psH", d)])
            P.op("act", "copy", s["H"][hn][:], s["psH"][t % 2],
                 reads=[("psH", d)], writes=[("H", d, hn)])

        def yout(d, t):
            s = st[d]
            cb = (t // NCOL) % 2
            jc = t % NCOL
            yb = (t // YCH) % 2
            jy = t % YCH
            P.op("pe", "matmul", s["psY"][yb][:, jy, :], s["H"][t % 2][:], s["rpad"][cb][:, jc, :],
                 start=True, stop=True,
                 reads=[("H", d, t % 2), ("rpad", d, cb)], writes=[("psY", d)])
            if jy == YCH - 1 or t == T - 1:
                n = jy + 1
                t0 = t - jy
                P.op("dve", "tensor_copy", s["ysb"][yb][:, :n, :], s["psY"][yb][:, :n, :],
                     reads=[("psY", d)], writes=[("ysb", d, yb)])
                P.dma("sp", y_d[d][:, t0:t0 + n, :], s["ysb"][yb][:, :n, :],
                      reads=[("ysb", d, yb)], writes=[("yout", d)])

        for d in range(2):
            load_rows(d, 0)
            load_cols(d, 0)
        for d in range(2):
            mmP(d, 0)
        for t in range(T):
            for d in range(2):
                if t >= 1:
                    yout(d, t - 1)
            for d in range(2):
                if t % SCAN_CH == 0 and (t // SCAN_CH + 1) * SCAN_CH < T:
                    load_rows(d, t // SCAN_CH + 1)
                if t % NCOL == 0 and (t // NCOL + 1) * NCOL < T:
                    load_cols(d, t // NCOL + 1)
            for d in range(2):
                if t + 1 < T:
                    mmP(d, t + 1)
                step(d, t)
        for d in range(2):
            yout(d, T - 1)
        P.finish([("yout", 0), ("yout", 1)])
    return nc, P


def build_mods():
    nc, stack, P = new_prog()
    with stack:
        cT_d = dram_in(nc, "cT", [128, 8, 3])
        w_d = dram_in(nc, "adaw", [2, 8, 128, 768])
        b_d = dram_in(nc, "adab", [2, 1, 768])
        o_d = dram_out(nc, "mod", [2, 3, 768])
        cT = sb(P, "cT_sb", [128, 8, 3])
        sg = sb(P, "sg", [128, 8, 3])
        sl = sb(P, "sl", [128, 8, 3])
        ones = sb(P, "ones1", [1, 3])
        P.dma("sp", cT[:], cT_d, writes=["cT"])
        P.op("act", "activation", sg[:], cT[:], AF.Sigmoid, reads=["cT"], writes=["sg"])
        P.op("dve", "tensor_tensor", sl[:], sg[:], cT[:], ALU.mult, reads=["sg", "cT"], writes=["sl"])
        P.op("dve", "memset", ones[:], 1.0, writes=["ones"])
        for l in range(2):
            w = sb(P, f"w{l}", [128, 8, 768])
            b = sb(P, f"b{l}", [1, 768])
            o = sb(P, f"o{l}", [3, 768])
            for k in range(8):
                P.dma("sp" if k % 2 == 0 else "pool", w[:, k, :], w_d[l, k], writes=[("w", l, k)])
            P.dma("sp", b[:], b_d[l], writes=[("b", l)])
            for h in range(2):
                pt = ps(P, f"ps{l}_{h}", [3, 384])
                cs = slice(h * 384, (h + 1) * 384)
                for k in range(8):
                    P.op("pe", "matmul", pt[:], sl[:, k, :], w[:, k, cs], start=(k == 0), stop=False,
                         reads=["sl", ("w", l, k)], writes=[("ps", l, h)])
                P.op("pe", "matmul", pt[:], ones[:], b[:, cs], start=False, stop=True,
                     reads=["ones", ("b", l)], writes=[("ps", l, h)])
                P.op("dve", "tensor_copy", o[:, cs], pt[:], reads=[("ps", l, h)], writes=[("o", l, h)])
            P.dma("sp", o_d[l], o[:], reads=[("o", l, 0), ("o", l, 1)], writes=[("out", l)])
        P.finish([("out", 0), ("out", 1)])
    return nc, P


def run_prog(nc, in_maps, n=8):
    res = run_bass_kernel_spmd(nc, in_maps, core_ids=list(range(n)))
    return res.results


def host_mods(inputs):
    cvec = np.concatenate([inputs["c"], inputs["c_ctx"][None, :]], axis=0).astype(np.float32)
    cT = np.ascontiguousarray(cvec.T.reshape(8, 128, 3).transpose(1, 0, 2))
    nc, P = build_mods()
    maps = []
    for i in range(8):
        cs = slice(i * 768, (i + 1) * 768)
        maps.append({
            "cT": cT,
            "adaw": np.ascontiguousarray(inputs["ada_w"][:, :, cs].reshape(2, 8, 128, 768)),
            "adab": np.ascontiguousarray(inputs["ada_b"][:, None, cs]),
        })
    res = run_prog(nc, maps)
    return np.concatenate([r["mod"] for r in res], axis=2)


NTOK = 2112
CHUNKS = [(0, 512, 0), (512, 512, 0), (1024, 512, 0), (1536, 512, 0), (2048, 64, 1)]
RMS_EPS = 1e-6


def col_layout(v):
    v = np.asarray(v, np.float32)
    lead = v.shape[:-1]
    return np.ascontiguousarray(np.moveaxis(v.reshape(lead + (8, 128)), -1, 0))


def emit_rmsnorm_mod(P, tag, xT, c0, n, gcol, scol, hT, ones, sq, pss, rstd, tmp, h0=None, hkey=None):
    if h0 is None:
        h0 = c0
    for k in range(8):
        P.op("act", "activation", sq[:, :n], xT[:, k, c0:c0 + n], AF.Square,
             reads=[(tag, "x", k)], writes=[(tag, "sq")])
        P.op("pe", "matmul", pss[:, :n], ones[:], sq[:, :n], start=(k == 0), stop=(k == 7),
             reads=[(tag, "sq"), "ones"], writes=[(tag, "pss")])
    P.op("dve", "tensor_scalar", rstd[:, :n], pss[:, :n], 1.0 / 1024.0, RMS_EPS, ALU.mult, ALU.add,
         reads=[(tag, "pss")], writes=[(tag, "rstd")])
    P.op("act", "activation", rstd[:, :n], rstd[:, :n], AF.Sqrt,
         reads=[(tag, "rstd")], writes=[(tag, "rstd")])
    P.op("dve", "reciprocal", rstd[:, :n], rstd[:, :n],
         reads=[(tag, "rstd")], writes=[(tag, "rstd")])
    for k in range(8):
        P.op("dve", "tensor_tensor", tmp[:, :n], xT[:, k, c0:c0 + n], rstd[:, :n], ALU.mult,
             reads=[(tag, "x", k), (tag, "rstd")], writes=[(tag, "tmp")])
        P.op("dve", "tensor_scalar", hT[:, k, h0:h0 + n], tmp[:, :n], gcol[:, k:k + 1], scol[:, k:k + 1],
             ALU.mult, ALU.add, reads=[(tag, "tmp"), (tag, "cols")], writes=[hkey if hkey is not None else (tag, "h", k)])


D_IN = 2944
NCH_IN = 23


def build_proj():
    nc, stack, P = new_prog()
    with stack:
        xT_d = dram_in(nc, "xT", [8, 128, NTOK])
        w_d = dram_in(nc, "w", [8, 128, D_IN])
        cols_d = dram_in(nc, "cols", [128, 2, 3, 8])
        o_d = dram_out(nc, "pT", [NCH_IN, 128, NTOK])
        xT = sb(P, "xT_sb", [128, 8, NTOK])
        hT = sb(P, "hT_sb", [128, 8, NTOK], BF16)
        w = sb(P, "w_sb", [128, 8, D_IN], BF16)
        cols = sb(P, "cols_sb", [128, 2, 3, 8])
        geff = sb(P, "geff", [128, 2, 8])
        ones = sb(P, "ones", [128, 128])
        sq = sb(P, "sq", [128, 512])
        tmp = sb(P, "tmp", [128, 512])
        rstd = sb(P, "rstd", [128, 512])
        pss = ps(P, "pss", [128, 512])
        pso = [ps(P, f"pso{i}", [128, 512]) for i in range(4)]
        osb = [sb(P, f"osb{i}", [128, 512]) for i in range(4)]
        P.op("dve", "memset", ones[:], 1.0, writes=["ones"])
        P.dma("sp", cols[:], cols_d, writes=["cols"])
        for k in range(8):
            P.dma("sp", xT[:, k, :], xT_d[k], writes=[("B", "x", k)])
            P.dma("pool", w[:, k, :], w_d[k], writes=[("w", k)])
        for s in range(2):
            P.op("dve", "scalar_tensor_tensor", geff[:, s, :], cols[:, s, 1, :], 1.0, cols[:, s, 0, :],
                 ALU.add, ALU.mult, reads=["cols"], writes=[("B", "cols")])
        cnt = 0
        for (c0, n, seg) in CHUNKS:
            emit_rmsnorm_mod(P, "B", xT, c0, n, geff[:, seg, :], cols[:, seg, 2, :], hT, ones, sq, pss, rstd, tmp)
            for j in range(NCH_IN):
                b = cnt % 4
                cnt += 1
                for k in range(8):
                    P.op("pe", "matmul", pso[b][:, :n], w[:, k, j * 128:(j + 1) * 128], hT[:, k, c0:c0 + n],
                         start=(k == 0), stop=(k == 7),
                         reads=[("w", k), ("B", "h", k)], writes=[("pso", b)])
                eng = "act" if j % 2 == 0 else "dve"
                if eng == "act":
                    P.op("act", "copy", osb[b][:, :n], pso[b][:, :n], reads=[("pso", b)], writes=[("osb", b)])
                else:
                    P.op("dve", "tensor_copy", osb[b][:, :n], pso[b][:, :n], reads=[("pso", b)], writes=[("osb", b)])
                P.dma("sp", o_d[j, :, c0:c0 + n], osb[b][:, :n], reads=[("osb", b)], writes=[("out", b)])
        P.finish([("out", b) for b in range(4)])
    return nc, P


def shard_tokens_T(xlat, xctx):
    outs = []
    fl = xlat.reshape(16384, -1)
    fc = xctx.reshape(512, -1)
    for i in range(8):
        t = np.concatenate([fl[i * 2048:(i + 1) * 2048], fc[i * 64:(i + 1) * 64]], axis=0)
        outs.append(np.ascontiguousarray(t.T))
    return outs


def unshard_tokens_T(outs):
    lat = np.concatenate([o[:, :2048].T for o in outs], axis=0)
    ctx = np.concatenate([o[:, 2048:].T for o in outs], axis=0)
    D = lat.shape[1]
    return lat.reshape(2, 8192, D), ctx.reshape(2, 256, D)


def mod_cols(mod_l, kinds, i, gain):
    b = i // 4
    out = np.zeros((128, 2, 3, 8), np.float32)
    for s, row in enumerate((b, 2)):
        out[:, s, 0, :] = col_layout(gain)
        out[:, s, 1, :] = col_layout(mod_l[row, kinds[0] * 1024:(kinds[0] + 1) * 1024])
        out[:, s, 2, :] = col_layout(mod_l[row, kinds[1] * 1024:(kinds[1] + 1) * 1024])
    return out


_CACHE = {}


def cached(name, fn, *a):
    key = (name,) + a
    if key not in _CACHE:
        _CACHE[key] = fn(*a)
    return _CACHE[key]


def host_proj(xlat, xctx, mod_l, gain, w_in_l):
    nc, P = cached("proj", build_proj)
    xs = shard_tokens_T(xlat, xctx)
    wl = np.ascontiguousarray(w_in_l.reshape(8, 128, D_IN))
    maps = []
    for i in range(8):
        maps.append({"xT": xs[i].reshape(8, 128, NTOK), "w": wl, "cols": mod_cols(mod_l, (1, 0), i, gain)})
    res = run_prog(nc, maps)
    return unshard_tokens_T([r["pT"].reshape(D_IN, NTOK) for r in res])


TSEQ = 8448
C_OUTS = ["r", "v", "kk", "nb0", "nb1", "kd0", "kd1", "s0", "s1", "g", "bonus"]
DEC_SCALE = float(-np.exp(-0.5))


def build_streams():
    nc, stack, P = new_prog()
    with stack:
        z_d = dram_in(nc, "zin", [9, 128, TSEQ])
        pc_d = dram_in(nc, "pc", [128, 16])
        mk_d = dram_in(nc, "masks", [128, 6])
        wl_d = dram_in(nc, "wl", [4, 128, 128])
        g2_d = dram_in(nc, "g2", [2, 128, 128])
        ob_d = dram_in(nc, "onesbd", [128, 128])
        outs = {n: dram_out(nc, "o_" + n, [128, TSEQ]) for n in C_OUTS}

        pc = sb(P, "pc_sb", [128, 16])
        mk = sb(P, "mk_sb", [128, 6])
        wl = sb(P, "wl_sb", [128, 4, 128])
        g2 = sb(P, "g2_sb", [128, 2, 128])
        ob = sb(P, "ob_sb", [128, 128])
        cs = sb(P, "cshift", [128, 9, 7])
        omka = sb(P, "omka", [128, 1])
        P.dma("sp", pc[:], pc_d, writes=["pc"])
        P.dma("sp", mk[:], mk_d, writes=["mk"])
        for i in range(4):
            P.dma("sp", wl[:, i, :], wl_d[i], writes=[("wl", i)])
        for i in range(2):
            P.dma("sp", g2[:, i, :], g2_d[i], writes=[("g2", i)])
        P.dma("sp", ob[:], ob_d, writes=["ob"])
        for s in range(9):
            P.op("dve", "tensor_scalar", cs[:, s, 0:1], pc[:, s:s + 1], -1.0, 1.0, ALU.mult, ALU.add,
                 reads=["pc"], writes=[("cs", s)])
            P.op("dve", "tensor_scalar", cs[:, s, 1:7], mk[:], pc[:, s:s + 1], None, ALU.mult,
                 reads=["pc", "mk"], writes=[("cs", s)])
        P.op("dve", "tensor_scalar", omka[:], pc[:, 10:11], -1.0, 1.0, ALU.mult, ALU.add, reads=["pc"], writes=["omka"])
        KK, KA, RK, W0F, W0B, A0F, A0B = 9, 10, 11, 12, 13, 14, 15

        NB = 1024
        zb = [sb(P, f"zb{i}", [128, 18, 64]) for i in range(2)]
        acc = {n: sb(P, "acc_" + n, [128, NB]) for n in ["r", "k", "v", "lw0", "lw1", "la0", "la1", "lg0", "lg1"]}
        t1 = sb(P, "t1", [128, NB])
        t2 = sb(P, "t2", [128, NB])
        kk = sb(P, "kk", [128, NB])
        kds = sb(P, "kds", [128, NB])
        ot = [sb(P, f"ot{i}", [128, NB]) for i in range(4)]
        pm = [ps(P, f"pm{i}", [128, 512]) for i in range(4)]
        names = ["r", "k", "v", "lw0", "lw1", "la0", "la1", "lg0", "lg1"]
        state = {"zb": 0, "ot": 0, "pm": 0}

        def out_tile():
            i = state["ot"] % 4
            state["ot"] += 1
            return ot[i], ("ot", i)

        def psum_tile():
            i = state["pm"] % 4
            state["pm"] += 1
            return pm[i], ("pm", i)

        def store(name, tile, key, t0, n):
            P.dma("sp", outs[name][:, t0:t0 + n], tile[:, :n], reads=[key], writes=[("out", name)], sem=("st", key))

        def shift(s, t0, n, ctx, j):
            bi = state["zb"] % 2
            state["zb"] += 1
            z = zb[bi]
            zk = ("zb", bi)
            a = acc[names[s]]
            ak = ("acc", s)
            q = "sp" if s % 2 == 0 else "pool"
            if ctx:
                zf = z[:].rearrange("p a b -> p (a b)")
                P.dma(q, zf[:, 0:256], z_d[s, :, 0:256], writes=[zk])
                P.op("dve", "tensor_scalar", a[:, :256], zf[:, 0:256], cs[:, s, 0:1], None, ALU.mult,
                     reads=[zk, ("cs", s)], writes=[ak])
                P.op("dve", "scalar_tensor_tensor", a[:, 1:256], zf[:, 0:255], cs[:, s, 5:6], a[:, 1:256],
                     ALU.mult, ALU.add, reads=[zk, ("cs", s), ak], writes=[ak])
                P.op("dve", "scalar_tensor_tensor", a[:, 0:255], zf[:, 1:256], cs[:, s, 6:7], a[:, 0:255],
                     ALU.mult, ALU.add, reads=[zk, ("cs", s), ak], writes=[ak])
                return
            r0 = 16 * j - 1
            lo = 1 if j == 0 else 0
            hi = 17 if j == 7 else 18
            if j == 0:
                P.op("dve", "memset", z[:, 0, :], 0.0, writes=[zk])
            if j == 7:
                P.op("dve", "memset", z[:, 17, :], 0.0, writes=[zk])
            src = z_d[s, :, 256 + (r0 + lo) * 64:256 + (r0 + hi) * 64].rearrange("p (a b) -> p a b", b=64)
            P.dma(q, z[:, lo:hi, :], src, writes=[zk])
            a3 = a[:].rearrange("p (a b) -> p a b", b=64)
            eng = "dve"
            P.op(eng, "tensor_scalar", a3[:, :, :], z[:, 1:17, :], cs[:, s, 0:1], None, ALU.mult,
                 reads=[zk, ("cs", s)], writes=[ak])
            for (dst, srcv, c) in ((a3[:, :, 1:64], z[:, 1:17, 0:63], 1), (a3[:, :, 0:63], z[:, 1:17, 1:64], 2),
                                   (a3[:, :, :], z[:, 0:16, :], 3), (a3[:, :, :], z[:, 2:18, :], 4)):
                P.op(eng, "scalar_tensor_tensor", dst, srcv, cs[:, s, c:c + 1], dst, ALU.mult, ALU.add,
                     reads=[zk, ("cs", s), ak], writes=[ak])

        chunks = [(0, 256, True, 0)] + [(256 + 1024 * j, 1024, False, j) for j in range(8)]
        for (t0, n, ctx, j) in chunks:
            for s in range(9):
                shift(s, t0, n, ctx, j)
            ncc = [(c0, min(512, n - c0)) for c0 in range(0, n, 512)]
            store("r", acc["r"], ("acc", 0), t0, n)
            store("v", acc["v"], ("acc", 2), t0, n)
            P.op("dve", "tensor_scalar", t1[:, :n], acc["k"][:, :n], pc[:, KK:KK + 1], None, ALU.mult,
                 reads=[("acc", 1), "pc"], writes=["t1"])
            P.op("act", "activation", t2[:, :n], t1[:, :n], AF.Square, reads=["t1"], writes=["t2"])
            for (c0, m) in ncc:
                pt, pk = psum_tile()
                P.op("pe", "matmul", pt[:, :m], ob[:], t2[:, c0:c0 + m], start=True, stop=True,
                     reads=["ob", "t2"], writes=[pk])
                P.op("act", "activation", kk[:, c0:c0 + m], pt[:, :m], AF.Sqrt, reads=[pk], writes=["kk"])
            P.op("dve", "tensor_scalar", kk[:, :n], kk[:, :n], 1e-12, None, ALU.max, reads=["kk"], writes=["kk"])
            P.op("dve", "reciprocal", kk[:, :n], kk[:, :n], reads=["kk"], writes=["kk"])
            P.op("dve", "tensor_tensor", kk[:, :n], kk[:, :n], t1[:, :n], ALU.mult, reads=["kk", "t1"], writes=["kk"])
            store("kk", kk, "kk", t0, n)
            for dn in range(2):
                P.op("act", "activation", t1[:, :n], acc[f"lw{dn}"][:, :n], AF.Tanh,
                     reads=[("acc", 3 + dn)], writes=["t1"])
                o, okey = out_tile()
                for (c0, m) in ncc:
                    pt, pk = psum_tile()
                    P.op("pe", "matmul", pt[:, :m], wl[:, dn, :], t1[:, c0:c0 + m], start=True, stop=True,
                         reads=[("wl", dn), "t1"], writes=[pk])
                    P.op("act", "activation", o[:, c0:c0 + m], pt[:, :m], AF.Sigmoid,
                         bias=pc[:, W0F + dn:W0F + dn + 1], reads=[pk, "pc"], writes=[okey])
                store(f"s{dn}", o, okey, t0, n)
                for (c0, m) in ncc:
                    pt, pk = psum_tile()
                    P.op("pe", "matmul", pt[:, :m], wl[:, 2 + dn, :], acc[f"la{dn}"][:, c0:c0 + m], start=True, stop=True,
                         reads=[("wl", 2 + dn), ("acc", 5 + dn)], writes=[pk])
                    P.op("act", "activation", t2[:, c0:c0 + m], pt[:, :m], AF.Sigmoid,
                         bias=pc[:, A0F + dn:A0F + dn + 1], reads=[pk, "pc"], writes=["t2"])
                o, okey = out_tile()
                P.op("dve", "scalar_tensor_tensor", o[:, :n], t2[:, :n], -1.0, kk[:, :n], ALU.mult, ALU.mult,
                     reads=["t2", "kk"], writes=[okey])
                store(f"nb{dn}", o, okey, t0, n)
                o, okey = out_tile()
                P.op("dve", "tensor_scalar", t2[:, :n], t2[:, :n], pc[:, KA:KA + 1], omka[:, 0:1], ALU.mult, ALU.add,
                     reads=["t2", "pc", "omka"], writes=["t2"])
                P.op("dve", "tensor_tensor", o[:, :n], t2[:, :n], acc["k"][:, :n], ALU.mult,
                     reads=["t2", ("acc", 1)], writes=[okey])
                store(f"kd{dn}", o, okey, t0, n)
                if dn == 0:
                    P.op("dve", "tensor_copy", kds[:, :n], o[:, :n], reads=[okey], writes=["kds"])
                else:
                    P.op("dve", "tensor_tensor", kds[:, :n], kds[:, :n], o[:, :n], ALU.add,
                         reads=[okey, "kds"], writes=["kds"])
            for b in range(2):
                P.op("act", "activation", acc[f"lg{b}"][:, :n], acc[f"lg{b}"][:, :n], AF.Sigmoid,
                     reads=[("acc", 7 + b)], writes=[("acc", 7 + b)])
            o, okey = out_tile()
            for (c0, m) in ncc:
                pt, pk = psum_tile()
                for b in range(2):
                    P.op("pe", "matmul", pt[:, :m], g2[:, b, :], acc[f"lg{b}"][:, c0:c0 + m], start=(b == 0), stop=(b == 1),
                         reads=[("g2", b), ("acc", 7 + b)], writes=[pk])
                P.op("act", "copy", o[:, c0:c0 + m], pt[:, :m], reads=[pk], writes=[okey])
            store("g", o, okey, t0, n)
            P.op("dve", "scalar_tensor_tensor", t1[:, :n], acc["r"][:, :n], pc[:, RK:RK + 1], kds[:, :n], ALU.mult, ALU.mult,
                 reads=[("acc", 0), "pc", "kds"], writes=["t1"])
            o, okey = out_tile()
            for (c0, m) in ncc:
                pt, pk = psum_tile()
                P.op("pe", "matmul", pt[:, :m], ob[:], t1[:, c0:c0 + m], start=True, stop=True,
                     reads=["ob", "t1"], writes=[pk])
                P.op("dve", "tensor_tensor", o[:, c0:c0 + m], pt[:, :m], acc["v"][:, c0:c0 + m], ALU.mult,
                     reads=[pk, ("acc", 2)], writes=[okey])
            store("bonus", o, okey, t0, n)
        P.finish([("out", n) for n in C_OUTS])
    return nc, P


def host_streams(plat, pctx, inp, l):
    nc, P = cached("streams", build_streams)
    pall = np.concatenate([pctx, plat], axis=1)
    masks = np.zeros((128, 6), np.float32)
    p = np.arange(128)
    for s in range(4):
        masks[:, s] = (p % 4 == s)
    masks[:, 4] = (p % 2 == 0)
    masks[:, 5] = (p % 2 == 1)
    onesbd = np.zeros((128, 128), np.float32)
    onesbd[:64, :64] = 1
    onesbd[64:, 64:] = 1

    def bh(v):
        return np.concatenate([v, v])

    def bd(m):
        o = np.zeros((128, 128), np.float32)
        o[:64, :64] = m
        o[64:, 64:] = m
        return o

    mu = inp["mu_shift"][l]
    maps = []
    for h in range(8):
        hs = slice(h * 64, (h + 1) * 64)
        zin = np.empty((9, 128, TSEQ), np.float32)
        for s, base in enumerate((0, 512, 1024)):
            zin[s] = pall[:, :, base + h * 64: base + (h + 1) * 64].transpose(0, 2, 1).reshape(128, TSEQ)
        for s, base in enumerate((1536, 1600, 1664, 1728)):
            zin[3 + s] = pall[:, :, base:base + 64].transpose(0, 2, 1).reshape(128, TSEQ)
        for b in range(2):
            zin[7 + b] = pall[b, :, 1792:1920].T
        pc = np.zeros((128, 16), np.float32)
        for s, base in enumerate((0, 512, 1024)):
            pc[:, s] = bh(mu[base + h * 64: base + (h + 1) * 64])
        for s, base in enumerate((1536, 1600, 1664, 1728)):
            pc[:, 3 + s] = bh(mu[base:base + 64])
        pc[:, 7] = mu[1792:1920]
        pc[:, 8] = mu[1792:1920]
        pc[:, 9] = bh(inp["k_k"][l][hs])
        pc[:, 10] = bh(inp["k_a"][l][hs])
        pc[:, 11] = bh(inp["r_k"][l][h])
        pc[:, 12] = bh(inp["decay_w0"][l][0][hs])
        pc[:, 13] = bh(inp["decay_w0"][l][1][hs])
        pc[:, 14] = bh(inp["iclr_a0"][l][0][hs])
        pc[:, 15] = bh(inp["iclr_a0"][l][1][hs])
        wl = np.stack([bd(inp["decay_w2"][l][0][:, hs]), bd(inp["decay_w2"][l][1][:, hs]),
                       bd(inp["iclr_a2"][l][0][:, hs]), bd(inp["iclr_a2"][l][1][:, hs])])
        g2 = np.zeros((2, 128, 128), np.float32)
        g2[0, :, :64] = inp["gate_g2"][l][:, hs]
        g2[1, :, 64:] = inp["gate_g2"][l][:, hs]
        maps.append({"zin": zin, "pc": pc, "masks": masks, "wl": wl, "g2": g2, "onesbd": onesbd})
    res = run_prog(nc, maps)
    return [{n: r["o_" + n] for n in C_OUTS} for r in res]


IDX_F = np.arange(TSEQ)
IDX_B = np.concatenate([np.arange(255, -1, -1), 256 + np.arange(8191, -1, -1)])


def host_scan(streams):
    T = TSEQ
    nc, P = cached("scan", build_scan, T)
    ident = np.eye(128, dtype=np.float32)
    maps = []
    for h in range(8):
        s = streams[h]
        m = {"ident": ident}
        for dn, idx in enumerate((IDX_F, IDX_B)):
            rows = np.zeros((2, T, 448), np.float32)
            rpad = np.zeros((128, T, 2), np.float32)
            for c in range(2):
                ps_ = slice(c * 64, (c + 1) * 64)
                rows[c, :, c * 64:(c + 1) * 64] = s["kk"][ps_][:, idx].T
                rows[c, :, 128 + c * 64:128 + (c + 1) * 64] = s[f"nb{dn}"][ps_][:, idx].T
                rows[c, :, 256 + c * 64:256 + (c + 1) * 64] = s[f"kd{dn}"][ps_][:, idx].T
                rows[c, :, 384:448] = s["v"][ps_][:, idx].T
                rpad[ps_, :, c] = s["r"][ps_][:, idx]
            m[f"rows{dn}"] = rows
            m[f"dcol{dn}"] = np.ascontiguousarray(s[f"d{dn}"][:, idx])
            m[f"rpad{dn}"] = rpad
        maps.append(m)
    res = run_prog(nc, maps)
    out = []
    for h in range(8):
        ys = []
        for dn, idx in enumerate((IDX_F, IDX_B)):
            y = res[h][f"y{dn}"]
            yfm = np.empty((128, T), np.float32)
            for c in range(2):
                yfm[c * 64:(c + 1) * 64][:, idx] = y[:, :, c]
            ys.append(yfm)
        out.append(tuple(ys))
    return out


GN_EPS = 64e-5


def build_readout():
    nc, stack, P = new_prog()
    with stack:
        yf_d = dram_in(nc, "yf", [128, TSEQ])
        yb_d = dram_in(nc, "yb", [128, TSEQ])
        bo_d = dram_in(nc, "bonus", [128, TSEQ])
        g_d = dram_in(nc, "g", [128, TSEQ])
        pc_d = dram_in(nc, "pc", [128, 2])
        ob_d = dram_in(nc, "obd64", [128, 128])
        o_d = dram_out(nc, "o", [128, TSEQ])
        pc = sb(P, "pc_sb", [128, 2])
        ob = sb(P, "ob_sb", [128, 128])
        P.dma("sp", pc[:], pc_d, writes=["pc"])
        P.dma("sp", ob[:], ob_d, writes=["ob"])
        NB = 512
        nbuf = 2
        bufs = []
        for i in range(nbuf):
            bufs.append({n: sb(P, f"{n}{i}", [128, NB]) for n in ["yf", "yb", "bo", "g", "t", "sq"]})
        pmu = [ps(P, f"pmu{i}", [128, NB]) for i in range(2)]
        pvar = [ps(P, f"pvar{i}", [128, NB]) for i in range(2)]
        nchunks = (TSEQ + NB - 1) // NB
        for c in range(nchunks):
            i = c % nbuf
            B = bufs[i]
            t0 = c * NB
            n = min(NB, TSEQ - t0)
            k = lambda nm: (nm, i)
            P.dma("sp", B["yf"][:, :n], yf_d[:, t0:t0 + n], writes=[k("yf")])
            P.dma("pool", B["yb"][:, :n], yb_d[:, t0:t0 + n], writes=[k("yb")])
            P.dma("sp", B["bo"][:, :n], bo_d[:, t0:t0 + n], writes=[k("bo")])
            P.dma("pool", B["g"][:, :n], g_d[:, t0:t0 + n], writes=[k("g")])
            P.op("dve", "tensor_tensor", B["yf"][:, :n], B["yf"][:, :n], B["yb"][:, :n], ALU.add,
                 reads=[k("yf"), k("yb")], writes=[k("yf")])
            P.op("pe", "matmul", pmu[i][:, :n], ob[:], B["yf"][:, :n], start=True, stop=True,
                 reads=["ob", k("yf")], writes=[k("pmu")])
            P.op("dve", "tensor_tensor", B["t"][:, :n], B["yf"][:, :n], pmu[i][:, :n], ALU.subtract,
                 reads=[k("yf"), k("pmu")], writes=[k("t")])
            P.op("act", "activation", B["sq"][:, :n], B["t"][:, :n], AF.Square, reads=[k("t")], writes=[k("sq")])
            P.op("pe", "matmul", pvar[i][:, :n], ob[:], B["sq"][:, :n], start=True, stop=True,
                 reads=["ob", k("sq")], writes=[k("pvar")])
            P.op("dve", "tensor_scalar", B["sq"][:, :n], pvar[i][:, :n], GN_EPS, None, ALU.add,
                 reads=[k("pvar")], writes=[k("sq")])
            P.op("act", "activation", B["sq"][:, :n], B["sq"][:, :n], AF.Sqrt, reads=[k("sq")], writes=[k("sq")])
            P.op("dve", "reciprocal", B["sq"][:, :n], B["sq"][:, :n], reads=[k("sq")], writes=[k("sq")])
            P.op("dve", "tensor_tensor", B["t"][:, :n], B["t"][:, :n], B["sq"][:, :n], ALU.mult,
                 reads=[k("t"), k("sq")], writes=[k("t")])
            P.op("dve", "tensor_scalar", B["t"][:, :n], B["t"][:, :n], pc[:, 0:1], pc[:, 1:2], ALU.mult, ALU.add,
                 reads=[k("t"), "pc"], writes=[k("t")])
            P.op("dve", "tensor_tensor", B["t"][:, :n], B["t"][:, :n], B["bo"][:, :n], ALU.add,
                 reads=[k("t"), k("bo")], writes=[k("t")])
            P.op("dve", "tensor_tensor", B["t"][:, :n], B["t"][:, :n], B["g"][:, :n], ALU.mult,
                 reads=[k("t"), k("g")], writes=[k("t")])
            P.dma("sp", o_d[:, t0:t0 + n], B["t"][:, :n], reads=[k("t")], writes=[("out", i)])
        P.finish([("out", i) for i in range(nbuf)])
    return nc, P


def host_readout(streams, ys, inp, l):
    nc, P = cached("readout", build_readout)
    obd = np.zeros((128, 128), np.float32)
    obd[:64, :64] = 1.0 / 64
    obd[64:, 64:] = 1.0 / 64
    maps = []
    for h in range(8):
        hs = slice(h * 64, (h + 1) * 64)
        pc = np.stack([np.concatenate([inp["gn_w"][l][hs]] * 2), np.concatenate([inp["gn_b"][l][hs]] * 2)], axis=1)
        maps.append({"yf": ys[h][0], "yb": ys[h][1], "bonus": streams[h]["bonus"], "g": streams[h]["g"],
                     "pc": np.ascontiguousarray(pc.astype(np.float32)), "obd64": obd})
    res = run_prog(nc, maps)
    o = np.empty((2, TSEQ, 512), np.float32)
    for h in range(8):
        o[:, :, h * 64:(h + 1) * 64] = res[h]["o"].reshape(2, 64, TSEQ).transpose(0, 2, 1)
    return o[:, 256:], o[:, :256]


def fourier_tables():
    k64 = np.arange(64)
    a64 = 2 * np.pi * np.outer(k64, k64) / 64.0
    C64, S64 = np.cos(a64), np.sin(a64)
    t64 = np.stack([np.concatenate([C64, -S64], 1), np.concatenate([S64, C64], 1),
                    np.concatenate([-S64, -C64], 1), np.concatenate([C64, -S64], 1)]).astype(np.float32)
    s1 = np.arange(64)
    t0 = np.arange(128)
    atw = 2 * np.pi * np.outer(s1, t0) / 8192.0
    Tc = np.concatenate([np.cos(atw)] * 2, 0).astype(np.float32)
    Ts = np.concatenate([np.sin(atw)] * 2, 0).astype(np.float32)
    k128 = np.arange(128)
    a128 = 2 * np.pi * np.outer(k128, k128) / 128.0
    sc = 1.0 / np.sqrt(8192.0 * 64.0)
    cs128 = np.stack([np.cos(a128) * sc, np.sin(a128) * sc]).astype(np.float32)
    k256 = np.arange(256)
    a256 = 2 * np.pi * np.outer(k256, k256) / 256.0
    sc2 = 1.0 / np.sqrt(256.0 * 64.0)
    cs256 = np.stack([np.cos(a256) * sc2, np.sin(a256) * sc2]).astype(np.float32).reshape(2, 2, 128, 256)
    return {"t64": t64, "Tc": Tc, "Ts": Ts, "cs128": cs128, "cs256": cs256, "ident": np.eye(128, dtype=np.float32)}


def build_fourier():
    nc, stack, P = new_prog()
    with stack:
        x_d = dram_in(nc, "xT", [64, 8192])
        xc_d = dram_in(nc, "xcT", [64, 256])
        t64_d = dram_in(nc, "t64", [4, 64, 128])
        tc_d = dram_in(nc, "Tc", [128, 128])
        ts_d = dram_in(nc, "Ts", [128, 128])
        cs128_d = dram_in(nc, "cs128", [2, 128, 128])
        cs256_d = dram_in(nc, "cs256", [2, 2, 128, 256])
        id_d = dram_in(nc, "ident", [128, 128])
        y_d = dram_out(nc, "y", [128, 4096])
        yc_d = dram_out(nc, "yc", [2, 128, 64])

        xT = sb(P, "xT_sb", [64, 8192])
        xc = sb(P, "xc_sb", [64, 256])
        t64 = sb(P, "t64_sb", [64, 4, 128])
        Tc = sb(P, "Tc_sb", [128, 128])
        Ts = sb(P, "Ts_sb", [128, 128])
        cs128 = sb(P, "cs128_sb", [128, 2, 128])
        cs256 = sb(P, "cs256_sb", [128, 2, 2, 256])
        ident = sb(P, "ident_sb", [128, 128])
        Zt = sb(P, "Zt", [64, 128, 128])
        Gp = sb(P, "Gp", [128, 128, 64])
        Gt = sb(P, "Gt", [128, 64, 128])
        tmpa = sb(P, "tmpa", [128, 512])
        tmpb = sb(P, "tmpb", [128, 512])
        pp = [ps(P, f"pp{i}", [128, 512]) for i in range(6)]
        P.dma("sp", xT[:], x_d, writes=["xT"])
        P.dma("pool", xc[:], xc_d, writes=["xc"])
        for i in range(4):
            P.dma("sp", t64[:, i, :], t64_d[i], writes=[("t64", i)])
        P.dma("sp", Tc[:], tc_d, writes=["Tc"])
        P.dma("sp", Ts[:], ts_d, writes=["Ts"])
        for i in range(2):
            P.dma("pool", cs128[:, i, :], cs128_d[i], writes=[("cs128", i)])
            for j in range(2):
                P.dma("pool", cs256[:, i, j, :], cs256_d[i, j], writes=[("cs256", i, j)])
        P.dma("sp", ident[:], id_d, writes=["ident"])

        xv = xT[:].rearrange("c (t1 t0) -> c t0 t1", t0=128)
        for q in range(32):
            pt = pp[q % 2]
            pk = ("pp", q % 2)
            for i in range(4):
                t0 = q * 4 + i
                P.op("pe", "matmul", pt[0:64, i * 128:(i + 1) * 128], xv[:, t0, :], t64[:, 0, :], start=True, stop=True,
                     reads=["xT", ("t64", 0)], writes=[pk])
            eng = "act" if q % 2 == 0 else "dve"
            dst = Zt[:, q * 4:(q + 1) * 4, :].rearrange("p a b -> p (a b)")
            if eng == "act":
                P.op("act", "copy", dst, pt[0:64, :], reads=[pk], writes=[("Zt", q // 2)])
            else:
                P.op("dve", "tensor_copy", dst, pt[0:64, :], reads=[pk], writes=[("Zt", q // 2)])
        for q in range(16):
            p1 = pp[2 + (q % 2) * 2]
            p2 = pp[3 + (q % 2) * 2]
            k1 = ("pp", 2 + (q % 2) * 2)
            k2 = ("pp", 3 + (q % 2) * 2)
            zre = Zt[:, q * 8:(q + 1) * 8, 0:64]
            zim = Zt[:, q * 8:(q + 1) * 8, 64:128]
            P.op("pe", "matmul", p1[:].rearrange("p (a b) -> p a b", b=64), t64[:, 0, :], zre, start=True, stop=False,
                 reads=[("t64", 0), ("Zt", q)], writes=[k1])
            P.op("pe", "matmul", p1[:].rearrange("p (a b) -> p a b", b=64), t64[:, 1, :], zim, start=False, stop=True,
                 reads=[("t64", 1), ("Zt", q)], writes=[k1])
            P.op("pe", "matmul", p2[:].rearrange("p (a b) -> p a b", b=64), t64[:, 2, :], zre, start=True, stop=False,
                 reads=[("t64", 2), ("Zt", q)], writes=[k2])
            P.op("pe", "matmul", p2[:].rearrange("p (a b) -> p a b", b=64), t64[:, 3, :], zim, start=False, stop=True,
                 reads=[("t64", 3), ("Zt", q)], writes=[k2])
            tcb = Tc[:, q * 8:(q + 1) * 8].unsqueeze(2).broadcast_to([128, 8, 64])
            tsb = Ts[:, q * 8:(q + 1) * 8].unsqueeze(2).broadcast_to([128, 8, 64])
            P.op("dve", "tensor_tensor", tmpa[:].rearrange("p (a b) -> p a b", b=64),
                 p1[:].rearrange("p (a b) -> p a b", b=64), tcb, ALU.mult, reads=[k1, "Tc"], writes=["tmpa"])
            P.op("dve", "tensor_tensor", tmpb[:].rearrange("p (a b) -> p a b", b=64),
                 p2[:].rearrange("p (a b) -> p a b", b=64), tsb, ALU.mult, reads=[k2, "Ts"], writes=["tmpb"])
            P.op("dve", "tensor_tensor", Gp[:, q * 8:(q + 1) * 8, :].rearrange("p a b -> p (a b)"), tmpa[:], tmpb[:], ALU.add,
                 reads=["tmpa", "tmpb"], writes=[("Gp", q)])
        for q in range(16):
            pt = pp[q % 2]
            pk = ("pp", q % 2)
            for i in range(4):
                c = q * 4 + i
                P.op("pe", "transpose", pt[:, i * 128:(i + 1) * 128], Gp[:, :, c], ident[:],
                     reads=[("Gp", j) for j in range(16)] + ["ident"], writes=[pk])
            dst = Gt[:, q * 4:(q + 1) * 4, :].rearrange("p a b -> p (a b)")
            if q % 2 == 0:
                P.op("act", "copy", dst, pt[:], reads=[pk], writes=[("Gt", q // 2)])
            else:
                P.op("dve", "tensor_copy", dst, pt[:], reads=[pk], writes=[("Gt", q // 2)])
        Y = Zt
        Yv = Gp[:, 0:64, :].rearrange("p s c -> p c s")
        for q in range(8):
            pt = pp[2 + (q % 2) * 2]
            pk = ("pp", 2 + (q % 2) * 2)
            P.op("pe", "matmul", pt[:].rearrange("p (a b) -> p a b", b=64), cs128[:, 0, :], Gt[:, q * 8:(q + 1) * 8, 0:64],
                 start=True, stop=False, reads=[("cs128", 0), ("Gt", q)], writes=[pk])
            P.op("pe", "matmul", pt[:].rearrange("p (a b) -> p a b", b=64), cs128[:, 1, :], Gt[:, q * 8:(q + 1) * 8, 64:128],
                 start=False, stop=True, reads=[("cs128", 1), ("Gt", q)], writes=[pk])
            P.op("dve", "tensor_copy", Yv[:, q * 8:(q + 1) * 8, :], pt[:].rearrange("p (a b) -> p a b", b=64),
                 reads=[pk] + [("Gt", j) for j in range(8)], writes=[("Gp", j) for j in range(16)])
        P.dma("sp", y_d, Gp[:, 0:64, :].rearrange("p a b -> p (a b)"), reads=[("Gp", j) for j in range(16)], writes=["yout"])
        AB = sb(P, "AB", [128, 2, 128])
        yc = sb(P, "yc_sb", [128, 2, 64])
        for m in range(2):
            pt = pp[m]
            P.op("pe", "matmul", pt[:, 0:128], xc[:, m * 128:(m + 1) * 128], t64[:, 0, :], start=True, stop=True,
                 reads=["xc", ("t64", 0)], writes=[("pp", m)])
            P.op("act", "copy", AB[:, m, :], pt[:, 0:128], reads=[("pp", m)], writes=[("AB", m)])
        for sc_ in range(2):
            pt = pp[2 + sc_]
            first = True
            for m in range(2):
                for i in range(2):
                    P.op("pe", "matmul", pt[:, 0:64], cs256[:, i, m, sc_ * 128:(sc_ + 1) * 128], AB[:, m, i * 64:(i + 1) * 64],
                         start=first, stop=(m == 1 and i == 1),
                         reads=[("cs256", i, m), ("AB", m)], writes=[("pp", 2 + sc_)])
                    first = False
            P.op("dve", "tensor_copy", yc[:, sc_, :], pt[:, 0:64], reads=[("pp", 2 + sc_)], writes=[("yc", sc_)])
            P.dma("sp", yc_d[sc_], yc[:, sc_, :], reads=[("yc", sc_)], writes=[("ycout", sc_)])
        P.finish(["yout", ("ycout", 0), ("ycout", 1)])
    return nc, P


def host_fourier(plat, pctx):
    nc, P = cached("fourier", build_fourier)
    tabs = fourier_tables()
    maps = []
    for i in range(8):
        b, g = i // 4, i % 4
        cs = slice(2688 + g * 64, 2688 + (g + 1) * 64)
        m = dict(tabs)
        m["xT"] = np.ascontiguousarray(plat[b, :, cs].T)
        m["xcT"] = np.ascontiguousarray(pctx[b, :, cs].T)
        maps.append(m)
    res = run_prog(nc, maps)
    lat = np.empty((2, 8192, 256), np.float32)
    ctx = np.empty((2, 256, 256), np.float32)
    for i in range(8):
        b, g = i // 4, i % 4
        lat[b, :, g * 64:(g + 1) * 64] = res[i]["y"].reshape(8192, 64)
        ctx[b, :, g * 64:(g + 1) * 64] = res[i]["yc"].reshape(256, 64)
    return lat, ctx


NHAL = 2116
BIG = 1.0e30


def build_g1():
    nc, stack, P = new_prog()
    with stack:
        x_d = dram_in(nc, "xT", [8, 128, NTOK])
        o_d = dram_in(nc, "oT", [4, 128, NTOK])
        cv_d = dram_in(nc, "cvT", [6, 128, NHAL])
        f_d = dram_in(nc, "fT", [2, 128, NTOK])
        wo_d = dram_in(nc, "wout", [8, 128, 1024])
        cols_d = dram_in(nc, "cols", [128, 2, 4, 8])
        cvp_d = dram_in(nc, "cvp", [128, 2, 4])
        fg_d = dram_in(nc, "fg", [128, 2])
        rw_d = dram_in(nc, "rw", [8, 128, 36])
        rb_d = dram_in(nc, "rb", [36, 1])
        id_d = dram_in(nc, "ident", [128, 128])
        xo_d = dram_out(nc, "xo", [8, 128, NTOK])
        h2_d = dram_out(nc, "h2", [8, 128, NTOK])
        gm_d = dram_out(nc, "gm", [NTOK, 32])

        xT = sb(P, "xT_sb", [128, 8, NTOK])
        catb = sb(P, "catb", [128, 8, NTOK], BF16)
        wo = sb(P, "wo_sb", [128, 8, 1024], BF16)
        cols = sb(P, "cols_sb", [128, 2, 4, 8])
        geff = sb(P, "geff", [128, 2, 8])
        cvp = sb(P, "cvp_sb", [128, 2, 4])
        fg = sb(P, "fg_sb", [128, 2])
        rw = sb(P, "rw_sb", [128, 8, 36])
        rb = sb(P, "rb_sb", [36, 1])
        ident = sb(P, "ident_sb", [128, 128])
        ones = sb(P, "ones", [128, 128])
        sq = sb(P, "sq", [128, 512])
        tmp = sb(P, "tmp", [128, 512])
        rstd = sb(P, "rstd", [128, 512])
        h2f = sb(P, "h2f", [128, 8, 512])
        cvin = sb(P, "cvin", [128, 6, 514])
        zc = sb(P, "zc", [128, 514])
        cacc = sb(P, "cacc", [128, 2, 512])
        fin = sb(P, "fin", [128, 2, 512])
        LT = sb(P, "LT", [36, 512])
        pss = ps(P, "pss", [128, 512])
        pso = [ps(P, f"pso{i}", [128, 512]) for i in range(2)]
        plg = ps(P, "plg", [36, 512])
        ptr = ps(P, "ptr", [128, 36])
        P.op("dve", "memset", ones[:], 1.0, writes=["ones"])
        P.dma("sp", cols[:], cols_d, writes=["cols"])
        P.dma("sp", cvp[:], cvp_d, writes=["cvp"])
        P.dma("sp", fg[:], fg_d, writes=["fg"])
        P.dma("sp", rb[:], rb_d, writes=["rb"])
        P.dma("sp", ident[:], id_d, writes=["ident"])
        for k in range(8):
            P.dma("sp", xT[:, k, :], x_d[k], writes=[("G", "x", k)])
            P.dma("pool", wo[:, k, :], wo_d[k], writes=[("wo", k)])
            P.dma("sp", rw[:, k, :], rw_d[k], writes=[("rw", k)])
        for k in range(4):
            P.dma("pool", catb[:, k, :], o_d[k], writes=[("cat", k)])
        for s in range(2):
            P.op("dve", "scalar_tensor_tensor", geff[:, s, :], cols[:, s, 2, :], 1.0, cols[:, s, 1, :],
                 ALU.add, ALU.mult, reads=["cols"], writes=[("G", "cols")])

        def branch_norm(src_tiles, srck, gain_cols, kbase, c0, n):
            for j in range(2):
                P.op("act", "activation", sq[:, :n], src_tiles[j], AF.Square, reads=[srck], writes=["sqb"])
                P.op("pe", "matmul", pss[:, :n], ones[:], sq[:, :n], start=(j == 0), stop=(j == 1),
                     reads=["sqb", "ones"], writes=["pss"])
            P.op("dve", "tensor_scalar", rstd[:, :n], pss[:, :n], 1.0 / 256.0, RMS_EPS, ALU.mult, ALU.add,
                 reads=["pss"], writes=["rstdb"])
            P.op("act", "activation", rstd[:, :n], rstd[:, :n], AF.Sqrt, reads=["rstdb"], writes=["rstdb"])
            P.op("dve", "reciprocal", rstd[:, :n], rstd[:, :n], reads=["rstdb"], writes=["rstdb"])
            for j in range(2):
                P.op("dve", "scalar_tensor_tensor", catb[:, kbase + j, c0:c0 + n], src_tiles[j], gain_cols[j], rstd[:, :n],
                     ALU.mult, ALU.mult, reads=[srck, "rstdb", "cvp", "fg"], writes=[("cat", kbase + j)])

        tile_idx = 0
        for (c0, n, seg) in CHUNKS:
            hc0 = c0 if seg == 0 else 2050
            for j in range(6):
                w = n if j < 2 else n + 2
                off = hc0 + (1 if j < 2 else 0)
                P.dma("sp" if j % 2 == 0 else "pool", cvin[:, j, :w], cv_d[j, :, off:off + w], writes=[("cvin", j)])
            for j in range(2):
                P.op("dve", "tensor_tensor", zc[:, :n + 2], cvin[:, 2 + j, :n + 2], cvin[:, 4 + j, :n + 2], ALU.mult,
                     reads=[("cvin", 2 + j), ("cvin", 4 + j)], writes=["zc"])
                P.op("dve", "tensor_scalar", cacc[:, j, :n], zc[:, 1:n + 1], cvp[:, j, 1:2], None, ALU.mult,
                     reads=["zc", "cvp"], writes=["cacc"])
                P.op("dve", "scalar_tensor_tensor", cacc[:, j, :n], zc[:, 0:n], cvp[:, j, 0:1], cacc[:, j, :n],
                     ALU.mult, ALU.add, reads=["zc", "cvp", "cacc"], writes=["cacc"])
                P.op("dve", "scalar_tensor_tensor", cacc[:, j, :n], zc[:, 2:n + 2], cvp[:, j, 2:3], cacc[:, j, :n],
                     ALU.mult, ALU.add, reads=["zc", "cvp", "cacc"], writes=["cacc"])
                P.op("dve", "tensor_tensor", cacc[:, j, :n], cacc[:, j, :n], cvin[:, j, :n], ALU.mult,
                     reads=["cacc", ("cvin", j)], writes=["cacc"])
            branch_norm([cacc[:, 0, :n], cacc[:, 1, :n]], "cacc", [cvp[:, 0, 3:4], cvp[:, 1, 3:4]], 4, c0, n)
            for j in range(2):
                P.dma("sp", fin[:, j, :n], f_d[j, :, c0:c0 + n], writes=["fin"], sem=("fin", j))
            branch_norm([fin[:, 0, :n], fin[:, 1, :n]], "fin", [fg[:, 0:1], fg[:, 1:2]], 6, c0, n)
            for j in range(8):
                pt = pso[j % 2]
                for k in range(8):
                    P.op("pe", "matmul", pt[:, :n], wo[:, k, j * 128:(j + 1) * 128], catb[:, k, c0:c0 + n],
                         start=(k == 0), stop=(k == 7), reads=[("wo", k), ("cat", k)], writes=[("pso", j % 2)])
                P.op("dve", "scalar_tensor_tensor", xT[:, j, c0:c0 + n], pt[:, :n], cols[:, seg, 0, j:j + 1], xT[:, j, c0:c0 + n],
                     ALU.mult, ALU.add, reads=[("pso", j % 2), "cols", ("G", "x", j)], writes=[("G", "x", j)])
                P.dma("sp", xo_d[j, :, c0:c0 + n], xT[:, j, c0:c0 + n], reads=[("G", "x", j)], writes=[("xo", j)])
            emit_rmsnorm_mod(P, "G", xT, c0, n, geff[:, seg, :], cols[:, seg, 3, :], h2f, ones, sq, pss, rstd, tmp,
                             h0=0, hkey="h2f")
            for k in range(8):
                P.dma("pool", h2_d[k, :, c0:c0 + n], h2f[:, k, :n], reads=["h2f"], writes=[("h2o", k)])
            for k in range(8):
                P.op("pe", "matmul", plg[:, :n], rw[:, k, :], h2f[:, k, :n], start=(k == 0), stop=(k == 7),
                     reads=[("rw", k), "h2f"], writes=["plg"])
            P.op("act", "activation", LT[:, :n], plg[:, :n], AF.Identity, bias=rb[:, 0:1], reads=["plg", "rb"], writes=["LT"])
            for j in range(0, n, 128):
                m = min(128, n - j)
                ti = tile_idx
                tile_idx += 1
                L = sb(P, f"L{ti}", [128, 36])
                W = sb(P, f"W{ti}", [128, 80])
                G = sb(P, f"Gm{ti}", [128, 32])
                lk, wk, gk = ("L", ti), ("W", ti), ("Gm", ti)
                P.op("pe", "transpose", ptr[:m, :], LT[:, j:j + m], ident[:36, :36], reads=["LT", "ident"], writes=["ptr"])
                P.op("dve", "tensor_copy", L[:m, :], ptr[:m, :], reads=["ptr"], writes=[lk])
                gmax, ngmax, seg_, pgt = W[:m, 0:1], W[:m, 1:2], W[:m, 2:3], W[:m, 3:4]
                ohg, eg = W[:m, 4:8], W[:m, 8:12]
                m1, m2, d12, s1, w1, w2 = W[:m, 12:13], W[:m, 13:14], W[:m, 14:15], W[:m, 15:16], W[:m, 16:17], W[:m, 17:18]
                lem = W[:m, 18:50]
                P.op("dve", "tensor_reduce", gmax, L[:m, 0:4], AX.X, ALU.max, reads=[lk], writes=[wk])
                P.op("dve", "tensor_scalar", ngmax, gmax, -1.0, None, ALU.mult, reads=[wk], writes=[wk])
                P.op("act", "activation", eg, L[:m, 0:4], AF.Exp, bias=ngmax, reads=[lk, wk], writes=[wk])
                P.op("dve", "tensor_reduce", seg_, eg, AX.X, ALU.add, reads=[wk], writes=[wk])
                P.op("dve", "reciprocal", pgt, seg_, reads=[wk], writes=[wk])
                P.op("dve", "tensor_scalar", ohg, L[:m, 0:4], gmax, None, ALU.is_equal, reads=[lk, wk], writes=[wk])
                P.op("dve", "tensor_scalar", ohg, ohg, -1.0, BIG, ALU.add, ALU.mult, reads=[wk], writes=[wk])
                P.op("dve", "tensor_tensor", lem.rearrange("p (g e) -> p g e", e=8), L[:m, 4:36].rearrange("p (g e) -> p g e", e=8),
                     ohg.unsqueeze(2).broadcast_to([m, 4, 8]), ALU.add, reads=[lk, wk], writes=[wk])
                P.op("dve", "tensor_reduce", m1, lem, AX.X, ALU.max, reads=[wk], writes=[wk])
                oh1 = W[:m, 50:82] if False else None
                O1 = sb(P, f"O1_{ti}", [128, 32])
                O2 = sb(P, f"O2_{ti}", [128, 32])
                o1k, o2k = ("O1", ti), ("O2", ti)
                P.op("dve", "tensor_scalar", O1[:m, :], lem, m1, None, ALU.is_equal, reads=[wk], writes=[o1k])
                P.op("dve", "scalar_tensor_tensor", lem, O1[:m, :], -BIG, lem, ALU.mult, ALU.add, reads=[o1k, wk], writes=[wk])
                P.op("dve", "tensor_reduce", m2, lem, AX.X, ALU.max, reads=[wk], writes=[wk])
                P.op("dve", "tensor_scalar", O2[:m, :], lem, m2, None, ALU.is_equal, reads=[wk], writes=[o2k])
                P.op("dve", "tensor_tensor", d12, m1, m2, ALU.subtract, reads=[wk], writes=[wk])
                P.op("act", "activation", s1, d12, AF.Sigmoid, reads=[wk], writes=[wk])
                P.op("dve", "tensor_tensor", w1, s1, pgt, ALU.mult, reads=[wk], writes=[wk])
                P.op("dve", "tensor_tensor", w2, pgt, w1, ALU.subtract, reads=[wk], writes=[wk])
                P.op("dve", "tensor_scalar", G[:m, :], O1[:m, :], w1, None, ALU.mult, reads=[o1k, wk], writes=[gk])
                P.op("dve", "scalar_tensor_tensor", G[:m, :], O2[:m, :], w2, G[:m, :], ALU.mult, ALU.add,
                     reads=[o2k, wk, gk], writes=[gk])
                P.dma("sp", gm_d[c0 + j:c0 + j + m, :], G[:m, :], reads=[gk], writes=[("gmo", ti)])
        P.finish([("xo", j) for j in range(8)] + [("h2o", k) for k in range(8)] + [("gmo", t) for t in range(tile_idx)])
    return nc, P


def halo_cols(lat, ctx, i):
    b, q = i // 4, i % 4
    C = lat.shape[2]
    out = np.zeros((C, NHAL), np.float32)
    lo, hi = q * 2048 - 1, (q + 1) * 2048 + 1
    a, e = max(lo, 0), min(hi, 8192)
    out[:, (a - lo):(a - lo) + (e - a)] = lat[b, a:e].T
    lo, hi = q * 64 - 1, (q + 1) * 64 + 1
    a, e = max(lo, 0), min(hi, 256)
    out[:, 2050 + (a - lo):2050 + (a - lo) + (e - a)] = ctx[b, a:e].T
    return out


def host_g1(xlat, xctx, olat, octx, plat, pctx, flat, fctx, mod_l, inp, l):
    nc, P = cached("g1", build_g1)
    xs = shard_tokens_T(xlat, xctx)
    os_ = shard_tokens_T(olat, octx)
    fs = shard_tokens_T(flat, fctx)
    wo = np.ascontiguousarray(inp["w_out"][l].reshape(8, 128, 1024))
    rwf = np.concatenate([inp["router_g_w"][l], inp["router_e_w"][l]], axis=1)
    rw = np.ascontiguousarray(rwf.reshape(8, 128, 36))
    rb = np.concatenate([inp["router_g_b"][l], inp["router_e_b"][l]])[:, None].astype(np.float32)
    cvp = np.zeros((128, 2, 4), np.float32)
    for j in range(2):
        for t in range(3):
            cvp[:, j, t] = inp["conv_w"][l][t, j * 128:(j + 1) * 128]
        cvp[:, j, 3] = inp["conv_gain"][l][j * 128:(j + 1) * 128]
    fg = np.ascontiguousarray(inp["four_gain"][l].reshape(2, 128).T)
    ident = np.eye(128, dtype=np.float32)
    maps = []
    for i in range(8):
        b = i // 4
        cols = np.zeros((128, 2, 4, 8), np.float32)
        for s, row in enumerate((b, 2)):
            cols[:, s, 0, :] = col_layout(mod_l[row, 2048:3072])
            cols[:, s, 1, :] = col_layout(inp["norm2_g"][l])
            cols[:, s, 2, :] = col_layout(mod_l[row, 4096:5120])
            cols[:, s, 3, :] = col_layout(mod_l[row, 3072:4096])
        cv = halo_cols(plat[:, :, 1920:2688], pctx[:, :, 1920:2688], i)
        maps.append({"xT": xs[i].reshape(8, 128, NTOK), "oT": os_[i].reshape(4, 128, NTOK),
                     "cvT": cv.reshape(6, 128, NHAL), "fT": fs[i].reshape(2, 128, NTOK), "wout": wo,
                     "cols": cols, "cvp": cvp, "fg": fg, "rw": rw, "rb": rb, "ident": ident})
    res = run_prog(nc, maps)
    xo = unshard_tokens_T([r["xo"].reshape(1024, NTOK) for r in res])
    h2 = [r["h2"] for r in res]
    gm = [r["gm"] for r in res]
    return xo, h2, gm


FCHUNKS = [(c0, min(256, NTOK - c0)) for c0 in range(0, NTOK, 256)]


def build_g2():
    nc, stack, P = new_prog()
    with stack:
        x_d = dram_in(nc, "xT", [8, 128, NTOK])
        h_d = dram_in(nc, "h2", [8, 128, NTOK])
        gt_d = dram_in(nc, "GT", [32, NTOK])
        wg_d = dram_in(nc, "wg", [32, 8, 128, 512])
        wu_d = dram_in(nc, "wu", [32, 8, 128, 512])
        wd_d = dram_in(nc, "wd", [32, 4, 128, 1024])
        cols_d = dram_in(nc, "cols", [128, 2, 8])
        fgn_d = dram_in(nc, "fgn", [128, 8])
        e32_d = dram_in(nc, "e32", [32, 32])
        xo_d = dram_out(nc, "xo", [8, 128, NTOK])
        xf_d = dram_out(nc, "xf", [8, 128, NTOK])

        xT = sb(P, "xT_sb", [128, 8, NTOK])
        h2b = sb(P, "h2b", [128, 8, NTOK], BF16)
        GT = sb(P, "GT_sb", [32, NTOK])
        wbuf = [sb(P, f"wbuf{i}", [128, 12288], BF16) for i in range(2)]
        cols = sb(P, "cols_sb", [128, 2, 8])
        fgn = sb(P, "fgn_sb", [128, 8])
        zero8 = sb(P, "zero8", [128, 8])
        e32 = sb(P, "e32_sb", [32, 32])
        ones = sb(P, "ones", [128, 128])
        sl = [sb(P, f"sl{i}", [128, 512]) for i in range(2)]
        hid = [sb(P, f"hid{i}", [128, 512]) for i in range(2)]
        hidg = [[sb(P, f"hidg{i}_{f}", [128, 512], BF16) for f in range(4)] for i in range(2)]
        sq = sb(P, "sq", [128, 256])
        tmp = sb(P, "tmp", [128, 256])
        rstd = sb(P, "rstd", [128, 256])
        hfin = sb(P, "hfin", [128, 8, 256])
        psg = [ps(P, f"psg{i}", [128, 512]) for i in range(2)]
        psu = [ps(P, f"psu{i}", [128, 512]) for i in range(2)]
        pgb = ps(P, "pgb", [128, 512])
        pso = [ps(P, f"pso{i}", [128, 512]) for i in range(2)]
        pss = ps(P, "pss", [128, 256])
        P.op("dve", "memset", ones[:], 1.0, writes=["ones"])
        P.op("dve", "memset", zero8[:], 0.0, writes=["zero8"])
        P.dma("sp", cols[:], cols_d, writes=["cols"])
        P.dma("sp", fgn[:], fgn_d, writes=["fgn"])
        P.dma("sp", e32[:], e32_d, writes=["e32"])
        P.dma("sp", GT[:], gt_d, writes=["GT"])
        for k in range(8):
            P.dma("sp", xT[:, k, :], x_d[k], writes=[("F", "x", k)])
            P.dma("pool", h2b[:, k, :], h_d[k], writes=[("h2b", k)])

        def load_w(e):
            b = e % 2
            wb = wbuf[b]
            P.dma("pool", wb[:, 0:4096].rearrange("p (k f) -> p k f", f=512), wg_d[e].rearrange("k p f -> p k f"),
                  writes=[("wg", b)])
            P.dma("pool", wb[:, 4096:8192].rearrange("p (k f) -> p k f", f=512), wu_d[e].rearrange("k p f -> p k f"),
                  writes=[("wu", b)])
            P.dma("pool", wb[:, 8192:12288].rearrange("p (k f) -> p k f", f=1024), wd_d[e].rearrange("k p f -> p k f"),
                  writes=[("wd", b)])

        load_w(0)
        load_w(1)
        state = {"cnt": 0}
        items = [(e, ch) for e in range(32) for ch in CHUNKS]

        def wviews(e):
            b = e % 2
            Wg = wbuf[b][:, 0:4096].rearrange("p (k f) -> p k f", f=512)
            Wu = wbuf[b][:, 4096:8192].rearrange("p (k f) -> p k f", f=512)
            Wd = wbuf[b][:, 8192:12288].rearrange("p (k f) -> p k f", f=1024)
            return b, Wg, Wu, Wd

        def GU(i):
            e, (c0, n, seg) = items[i]
            b, Wg, Wu, Wd = wviews(e)
            hs = i % 2
            P.op("pe", "matmul", pgb[:, :n], e32[:, e:e + 1].broadcast_to([32, 128]), GT[:, c0:c0 + n], start=True, stop=True,
                 reads=["e32", "GT"], writes=["pgb"])
            for f in range(4):
                j = state["cnt"] % 2
                state["cnt"] += 1
                for k in range(8):
                    P.op("pe", "matmul", psg[j][:, :n], Wg[:, k, f * 128:(f + 1) * 128], h2b[:, k, c0:c0 + n],
                         start=(k == 0), stop=(k == 7), reads=[("wg", b), ("h2b", k)], writes=[("psg", j)])
                for k in range(8):
                    P.op("pe", "matmul", psu[j][:, :n], Wu[:, k, f * 128:(f + 1) * 128], h2b[:, k, c0:c0 + n],
                         start=(k == 0), stop=(k == 7), reads=[("wu", b), ("h2b", k)], writes=[("psu", j)])
                P.op("act", "activation", sl[j][:, :n], psg[j][:, :n], AF.Silu, reads=[("psg", j)], writes=[("sl", j)])
                P.op("dve", "tensor_tensor", hid[j][:, :n], sl[j][:, :n], psu[j][:, :n], ALU.mult,
                     reads=[("sl", j), ("psu", j)], writes=[("hid", j)])
                P.op("dve", "tensor_tensor", hidg[hs][f][:, :n], hid[j][:, :n], pgb[:, :n], ALU.mult,
                     reads=[("hid", j), "pgb"], writes=[("hidg", hs, f)])

        def D(i):
            e, (c0, n, seg) = items[i]
            b, Wg, Wu, Wd = wviews(e)
            hs = i % 2
            for j in range(8):
                pt = pso[j % 2]
                for f in range(4):
                    P.op("pe", "matmul", pt[:, :n], Wd[:, f, j * 128:(j + 1) * 128], hidg[hs][f][:, :n],
                         start=(f == 0), stop=(f == 3), reads=[("wd", b), ("hidg", hs, f)], writes=[("pso", j % 2)])
                P.op("dve", "scalar_tensor_tensor", xT[:, j, c0:c0 + n], pt[:, :n], cols[:, seg, j:j + 1], xT[:, j, c0:c0 + n],
                     ALU.mult, ALU.add, reads=[("pso", j % 2), "cols", ("F", "x", j)], writes=[("F", "x", j)])

        GU(0)
        for i in range(len(items)):
            if i + 1 < len(items):
                GU(i + 1)
            D(i)
            e, ch = items[i]
            if ch is CHUNKS[-1] and e + 2 < 32:
                load_w(e + 2)
        for k in range(8):
            P.dma("sp", xo_d[k], xT[:, k, :], reads=[("F", "x", k)], writes=[("xo", k)])
        for (c0, n) in FCHUNKS:
            emit_rmsnorm_mod(P, "F", xT, c0, n, fgn, zero8, hfin, ones, sq, pss, rstd, tmp, h0=0, hkey="hfin")
            for k in range(8):
                P.dma("sp" if k % 2 == 0 else "pool", xf_d[k, :, c0:c0 + n], hfin[:, k, :n], reads=["hfin"], writes=[("xf", k)])
        P.finish([("xo", k) for k in range(8)] + [("xf", k) for k in range(8)])
    return nc, P


def host_g2(xlat, xctx, h2, gm, mod_l, inp, l):
    nc, P = cached("g2", build_g2)
    xs = shard_tokens_T(xlat, xctx)
    wg = np.ascontiguousarray(inp["exp_gate"][l].reshape(32, 8, 128, 512))
    wu = np.ascontiguousarray(inp["exp_up"][l].reshape(32, 8, 128, 512))
    wd = np.ascontiguousarray(inp["exp_down"][l].reshape(32, 4, 128, 1024))
    fgn = col_layout(inp["final_g"])
    e32 = np.eye(32, dtype=np.float32)
    maps = []
    for i in range(8):
        b = i // 4
        cols = np.zeros((128, 2, 8), np.float32)
        cols[:, 0, :] = col_layout(mod_l[b, 5120:6144])
        cols[:, 1, :] = col_layout(mod_l[2, 5120:6144])
        maps.append({"xT": xs[i].reshape(8, 128, NTOK), "h2": h2[i], "GT": np.ascontiguousarray(gm[i].T),
                     "wg": wg, "wu": wu, "wd": wd, "cols": cols, "fgn": fgn, "e32": e32})
    res = run_prog(nc, maps)
    xo = unshard_tokens_T([r["xo"].reshape(1024, NTOK) for r in res])
    xf = unshard_tokens_T([r["xf"].reshape(1024, NTOK) for r in res])
    return xo, xf


def kernel(**inputs):
    inp = {k: np.asarray(v, dtype=np.float32) for k, v in inputs.items()}
    mod = host_mods(inp)
    xlat, xctx = inp["x"], inp["ctx"]
    xf = None
    for l in range(2):
        plat, pctx = host_proj(xlat, xctx, mod[l], inp["norm1_g"][l], inp["w_in"][l])
        streams = host_streams(plat, pctx, inp, l)
        ys = host_scan2(streams)
        olat, octx = host_readout(streams, ys, inp, l)
        flat, fctx = host_fourier(plat, pctx)
        (xlat, xctx), h2, gm = host_g1(xlat, xctx, olat, octx, plat, pctx, flat, fctx, mod[l], inp, l)
        (xlat, xctx), xf = host_g2(xlat, xctx, h2, gm, mod[l], inp, l)
    return np.ascontiguousarray(xf[0].astype(np.float32))


def scan2_consts():
    p = np.arange(128) % 64
    t = np.arange(64)
    Ms = (p[:, None] < t[None, :]).astype(np.float32)
    Mi = (p[:, None] <= t[None, :]).astype(np.float32)
    MsT = (p[:, None] > t[None, :]).astype(np.float32)
    return {"ident": np.eye(128, dtype=np.float32), "M5": np.concatenate([Ms, Mi, Ms, Mi, MsT], 1)}


def build_scan2(T):
    assert T % 256 == 0
    NS = T // 256
    NCHK = T // 64
    nc, stack, P = new_prog()
    with stack:
        in_d = [dram_in(nc, f"in{d}", [6, 128, T]) for d in range(2)]
        id_d = dram_in(nc, "ident", [128, 128])
        m5_d = dram_in(nc, "M5", [128, 320])
        y_d = [dram_out(nc, f"y{d}", [2, T, 64]) for d in range(2)]
        ident = sb(P, "ident_sb", [128, 128])
        M5 = sb(P, "M5_sb", [128, 320])
        P.dma("sp", ident[:], id_d, writes=["ident"])
        P.dma("sp", M5[:], m5_d, writes=["M5"])
        S = []
        for d in range(2):
            s = {}
            s["IN"] = [sb(P, f"IN{d}_{i}", [128, 6, 256]) for i in range(2)]
            for nm in ["cA", "cB", "Dm", "Dinv", "Dprev", "kkt", "nbt", "kt", "rt", "nbh", "kdh"]:
                s[nm] = [sb(P, f"{nm}{d}_{i}", [128, 256]) for i in range(2)]
            s["GG"] = [sb(P, f"GG{d}_{i}", [128, 5, 128]) for i in range(2)]
            for nm in ["P0", "P1", "Q0", "Q1", "Z0", "Z1", "ZT0", "ZT1", "NBT", "KDT"]:
                s[nm] = [sb(P, f"{nm}_{d}_{i}", [128, 128]) for i in range(2)]
            s["VT"] = [sb(P, f"VT{d}_{i}", [128, 64]) for i in range(2)]
            s["XY0"] = [sb(P, f"XY0{d}_{i}", [128, 2, 64]) for i in range(2)]
            s["H"] = [sb(P, f"Hs{d}_{i}", [128, 64]) for i in range(2)]
            s["X"] = sb(P, f"Xs{d}", [128, 64])
            s["W"] = sb(P, f"Ws{d}", [128, 64])
            s["Y"] = [sb(P, f"Ys{d}_{i}", [128, 64]) for i in range(2)]
            s["ppA"] = ps(P, f"ppA{d}", [128, 512])
            s["ppB"] = [ps(P, f"ppB{d}_{i}", [128, 512]) for i in range(2)]
            s["psB"] = ps(P, f"psB{d}", [128, 512])
            s["ppc"] = 0
            S.append(s)
            P.op("dve", "memset", s["H"][0][:], 0.0, writes=[(d, "H", 0)])
            for i in range(2):
                P.op("dve", "memset", s["GG"][i][:], 0.0, writes=[(d, "GG", i)])
                P.op("dve", "memset", s["NBT"][i][:], 0.0, writes=[(d, "NBT", i)])
                P.op("dve", "memset", s["KDT"][i][:], 0.0, writes=[(d, "KDT", i)])

        def k_(d, *a):
            return (d,) + a

        def elementwise(d, sidx):
            s = S[d]
            pi = sidx % 2
            IN = s["IN"][pi]
            kin = k_(d, "IN", pi)
            P.dma("sp" if d == 0 else "pool", IN[:], in_d[d][:, :, sidx * 256:(sidx + 1) * 256].rearrange("q p t -> p q t"),
                  writes=[kin])
            yield
            cA, cB = s["cA"][pi], s["cB"][pi]
            ka, kb = k_(d, "cA", pi), k_(d, "cB", pi)
            P.op("dve", "tensor_scalar", cA[:], IN[:, 4, :], DEC_SCALE, None, ALU.mult, reads=[kin], writes=[ka])
            P.op("act", "copy", s["Dprev"][pi][:], cA[:], reads=[ka], writes=[k_(d, "Dprev", pi)])
            src, dst, ks, kd_ = cA, cB, ka, kb
            for sh in (1, 2, 4, 8, 16, 32):
                s3 = src[:].rearrange("p (c t) -> p c t", t=64)
                d3 = dst[:].rearrange("p (c t) -> p c t", t=64)
                P.op("dve", "tensor_tensor", d3[:, :, sh:], s3[:, :, sh:], s3[:, :, :64 - sh], ALU.add, reads=[ks], writes=[kd_])
                P.op("act", "copy", d3[:, :, :sh], s3[:, :, :sh], reads=[ks], writes=[kd_])
                src, dst, ks, kd_ = dst, src, kd_, ks
                yield
            LD, kld = src, ks
            Dm, Dinv, Dprev = s["Dm"][pi], s["Dinv"][pi], s["Dprev"][pi]
            kDm, kDinv, kDprev = k_(d, "Dm", pi), k_(d, "Dinv", pi), k_(d, "Dprev", pi)
            P.op("act", "activation", Dm[:], LD[:], AF.Exp, reads=[kld], writes=[kDm])
            P.op("act", "activation", Dinv[:], LD[:], AF.Exp, scale=-1.0, reads=[kld], writes=[kDinv])
            P.op("dve", "tensor_tensor", Dprev[:], LD[:], Dprev[:], ALU.subtract, reads=[kld, kDprev], writes=[kDprev])
            P.op("act", "activation", Dprev[:], Dprev[:], AF.Exp, reads=[kDprev], writes=[kDprev])
            yield
            P.op("dve", "tensor_tensor", s["kkt"][pi][:], IN[:, 0, :], Dprev[:], ALU.mult, reads=[kin, kDprev], writes=[k_(d, "kkt", pi)])
            P.op("dve", "tensor_tensor", s["nbt"][pi][:], IN[:, 1, :], Dinv[:], ALU.mult, reads=[kin, kDinv], writes=[k_(d, "nbt", pi)])
            yield
            P.op("dve", "tensor_tensor", s["kt"][pi][:], IN[:, 2, :], Dinv[:], ALU.mult, reads=[kin, kDinv], writes=[k_(d, "kt", pi)])
            P.op("dve", "tensor_tensor", s["rt"][pi][:], IN[:, 3, :], Dm[:], ALU.mult, reads=[kin, kDm], writes=[k_(d, "rt", pi)])
            yield
            dcb = Dm[:].rearrange("p (c t) -> p c t", t=64)[:, :, 63:64].broadcast_to([128, 4, 64])
            P.op("dve", "tensor_tensor", s["nbh"][pi][:].rearrange("p (c t) -> p c t", t=64),
                 s["nbt"][pi][:].rearrange("p (c t) -> p c t", t=64), dcb, ALU.mult,
                 reads=[k_(d, "nbt", pi), kDm], writes=[k_(d, "nbh", pi)])
            P.op("dve", "tensor_tensor", s["kdh"][pi][:].rearrange("p (c t) -> p c t", t=64),
                 s["kt"][pi][:].rearrange("p (c t) -> p c t", t=64), dcb, ALU.mult,
                 reads=[k_(d, "kt", pi), kDm], writes=[k_(d, "kdh", pi)])
            yield

        def ppB_next(d):
            s = S[d]
            i = s["ppc"] % 2
            s["ppc"] += 1
            return s["ppB"][i], k_(d, "ppB", i)

        def block_prep(d, ci):
            s = S[d]
            sidx, lc = ci // 4, ci % 4
            pi, gi = sidx % 2, ci % 2
            tk = slice(lc * 64, (lc + 1) * 64)
            IN = s["IN"][pi]
            ppA, kA = s["ppA"], k_(d, "ppA")
            kkt, nbt, kt, rt = s["kkt"][pi], s["nbt"][pi], s["kt"][pi], s["rt"][pi]
            kkk, knb, kkd, krt = k_(d, "kkt", pi), k_(d, "nbt", pi), k_(d, "kt", pi), k_(d, "rt", pi)
            for b in range(2):
                bh = slice(b * 64, (b + 1) * 64)
                P.op("pe", "matmul", ppA[bh, 0:64], s["nbh"][pi][bh, tk], ident[bh, bh], start=True, stop=True, reads=[k_(d, "nbh", pi), "ident"], writes=[kA])
                P.op("pe", "matmul", ppA[bh, 64:128], s["kdh"][pi][bh, tk], ident[bh, bh], start=True, stop=True, reads=[k_(d, "kdh", pi), "ident"], writes=[kA])
                P.op("pe", "matmul", ppA[bh, 128:192], IN[bh, 5, tk], ident[bh, bh], start=True, stop=True, reads=[k_(d, "IN", pi), "ident"], writes=[kA])
            for b in range(2):
                bh = slice(b * 64, (b + 1) * 64)
                P.op("act", "copy", s["NBT"][gi][bh, b * 64:(b + 1) * 64], ppA[bh, 0:64], reads=[kA], writes=[k_(d, "NBT", gi)])
                P.op("act", "copy", s["KDT"][gi][bh, b * 64:(b + 1) * 64], ppA[bh, 64:128], reads=[kA], writes=[k_(d, "KDT", gi)])
            P.op("act", "copy", s["VT"][gi][:], ppA[:, 128:192], reads=[kA], writes=[k_(d, "VT", gi)])
            yield
            for b in range(2):
                bh = slice(b * 64, (b + 1) * 64)
                for q, (lh, lk, rh, rk) in enumerate(((nbt, knb, kkt, kkk), (nbt, knb, rt, krt), (kt, kkd, kkt, kkk),
                                                      (kt, kkd, rt, krt), (kkt, kkk, nbt, knb))):
                    P.op("pe", "matmul", ppA[bh, 192 + q * 64:192 + (q + 1) * 64], lh[bh, tk], rh[bh, tk], start=True, stop=True,
                         reads=[lk, rk], writes=[kA])
            for b in range(2):
                bh = slice(b * 64, (b + 1) * 64)
                P.op("dve", "tensor_tensor", s["GG"][gi][bh, :, b * 64:(b + 1) * 64],
                     ppA[bh, 192:512].rearrange("p (q t) -> p q t", t=64), M5[bh, :].rearrange("p (q t) -> p q t", t=64), ALU.mult,
                     reads=[kA, "M5"], writes=[k_(d, "GG", gi)])
            yield
            GG, kGG = s["GG"][gi], k_(d, "GG", gi)
            pt, pk = ppB_next(d)
            P.op("pe", "matmul", pt[:, 0:64], GG[:, 2, :], s["VT"][gi][:], start=True, stop=True, reads=[kGG, k_(d, "VT", gi)], writes=[pk])
            P.op("pe", "matmul", pt[:, 64:128], GG[:, 3, :], s["VT"][gi][:], start=True, stop=True, reads=[kGG, k_(d, "VT", gi)], writes=[pk])
            P.op("act", "copy", s["XY0"][gi][:].rearrange("p k v -> p (k v)"), pt[:, 0:128], reads=[pk], writes=[k_(d, "XY0", gi)])
            yield
            Pc, kP = GG[:, 0, :], kGG
            Qc, kQ = GG[:, 4, :], kGG
            Zc, kZ = s["Z0"][gi][:], k_(d, "Z0", gi)
            ZTc, kZT = s["ZT0"][gi][:], k_(d, "ZT0", gi)
            P.op("dve", "tensor_tensor", Zc, Pc, ident[:], ALU.add, reads=[kP, "ident"], writes=[kZ])
            P.op("dve", "tensor_tensor", ZTc, Qc, ident[:], ALU.add, reads=[kQ, "ident"], writes=[kZT])
            yield
            for j in range(1, 6):
                last = j == 5
                Pn, kPn = s[f"P{j % 2}"][gi][:], k_(d, f"P{j % 2}", gi)
                Qn, kQn = s[f"Q{j % 2}"][gi][:], k_(d, f"Q{j % 2}", gi)
                Zn, kZn = s[f"Z{j % 2}"][gi][:], k_(d, f"Z{j % 2}", gi)
                ZTn, kZTn = s[f"ZT{j % 2}"][gi][:], k_(d, f"ZT{j % 2}", gi)
                pt, pk = ppB_next(d)
                P.op("pe", "matmul", pt[:, 0:128], Qc, Pc, start=True, stop=True, reads=[kQ, kP], writes=[pk])
                P.op("act", "copy", Pn, pt[:, 0:128], reads=[pk], writes=[kPn])
                if not last:
                    pt2, pk2 = ppB_next(d)
                    P.op("pe", "matmul", pt2[:, 0:128], Pc, Qc, start=True, stop=True, reads=[kQ, kP], writes=[pk2])
                    P.op("act", "copy", Qn, pt2[:, 0:128], reads=[pk2], writes=[kQn])
                yield
                pt, pk = ppB_next(d)
                P.op("pe", "matmul", pt[:, 0:128], ZTc, Pn, start=True, stop=True, reads=[kZT, kPn], writes=[pk])
                P.op("dve", "tensor_tensor", Zn, pt[:, 0:128], Zc, ALU.add, reads=[pk, kZ], writes=[kZn])
                if not last:
                    pt2, pk2 = ppB_next(d)
                    P.op("pe", "matmul", pt2[:, 0:128], Zc, Qn, start=True, stop=True, reads=[kZ, kQn], writes=[pk2])
                    P.op("dve", "tensor_tensor", ZTn, pt2[:, 0:128], ZTc, ALU.add, reads=[pk2, kZT], writes=[kZTn])
                yield
                Pc, kP, Qc, kQ, Zc, kZ, ZTc, kZT = Pn, kPn, Qn, kQn, Zn, kZn, ZTn, kZTn

        def seq_chunk(d, ci):
            s = S[d]
            sidx, lc = ci // 4, ci % 4
            pi, gi = sidx % 2, ci % 2
            tk = slice(lc * 64, (lc + 1) * 64)
            kkt, rt, Dm = s["kkt"][pi], s["rt"][pi], s["Dm"][pi]
            kkk, krt, kDm = k_(d, "kkt", pi), k_(d, "rt", pi), k_(d, "Dm", pi)
            VT, kVT = s["VT"][gi], k_(d, "VT", gi)
            GG, kGG = s["GG"][gi], k_(d, "GG", gi)
            XY0, kXY = s["XY0"][gi], k_(d, "XY0", gi)
            TT, kTT = s["Z1"][gi], k_(d, "Z1", gi)
            ppA, kA = s["ppA"], k_(d, "ppA")
            psB, kB = s["psB"], k_(d, "psB")
            X, W = s["X"], s["W"]
            kX, kW = k_(d, "X"), k_(d, "W")
            Hold, Hnew = s["H"][ci % 2], s["H"][(ci + 1) % 2]
            kHo, kHn = k_(d, "H", ci % 2), k_(d, "H", (ci + 1) % 2)
            for b in range(2):
                bh = slice(b * 64, (b + 1) * 64)
                P.op("pe", "matmul", ppA[bh, 0:64], kkt[bh, tk], Hold[bh, :], start=True, stop=True, reads=[kkk, kHo], writes=[kA])
                P.op("pe", "matmul", ppA[bh, 64:128], rt[bh, tk], Hold[bh, :], start=True, stop=True, reads=[krt, kHo], writes=[kA])
            P.op("dve", "tensor_tensor", X[:], ppA[:, 0:64], XY0[:, 0, :], ALU.add, reads=[kA, kXY], writes=[kX])
            Ysb, kYs = s["Y"][ci % 2], k_(d, "Ysb", ci % 2)
            P.op("dve", "tensor_tensor", Ysb[:], ppA[:, 64:128], XY0[:, 1, :], ALU.add, reads=[kA, kXY], writes=[kYs])
            yield
            P.op("pe", "matmul", psB[:, 0:64], TT[:], X[:], start=True, stop=True, reads=[kTT, kX], writes=[kB])
            P.op("act", "copy", W[:], psB[:, 0:64], reads=[kB], writes=[kW])
            yield
            P.op("pe", "matmul", psB[:, 64:128], s["KDT"][gi][:], VT[:], start=True, stop=False, reads=[k_(d, "KDT", gi), kVT], writes=[kB])
            P.op("pe", "matmul", psB[:, 64:128], s["NBT"][gi][:], W[:], start=False, stop=True, reads=[k_(d, "NBT", gi), kW], writes=[kB])
            P.op("pe", "matmul", psB[:, 128:192], GG[:, 1, :], W[:], start=True, stop=True, reads=[kGG, kW], writes=[kB])
            P.op("dve", "scalar_tensor_tensor", Hnew[:], Hold[:], Dm[:, tk.stop - 1:tk.stop], psB[:, 64:128], ALU.mult, ALU.add,
                 reads=[kHo, kDm, kB], writes=[kHn])
            P.op("dve", "tensor_tensor", Ysb[:], psB[:, 128:192], Ysb[:], ALU.add, reads=[kB, kYs], writes=[kYs])
            t0 = ci * 64
            for b in range(2):
                P.dma("sp", y_d[d][b, t0:t0 + 64, :], Ysb[b * 64:(b + 1) * 64, :], reads=[kYs], writes=[k_(d, "yout", ci % 2, b)])
            yield

        def run_interleaved(gens):
            gens = [g for g in gens if g is not None]
            while gens:
                nxt = []
                for g in gens:
                    try:
                        next(g)
                        nxt.append(g)
                    except StopIteration:
                        pass
                gens = nxt

        def chain(*gs):
            for g in gs:
                yield from g

        run_interleaved([chain(elementwise(d, 0), block_prep(d, 0)) for d in range(2)])
        for ci in range(NCHK):
            gens = []
            for d in range(2):
                gens.append(seq_chunk(d, ci))
                if ci + 1 < NCHK:
                    if (ci + 1) % 4 == 0:
                        gens.append(chain(elementwise(d, (ci + 1) // 4), block_prep(d, ci + 1)))
                    else:
                        gens.append(block_prep(d, ci + 1))
            run_interleaved(gens)
        P.finish([k_(d, "yout", yb, b) for d in range(2) for yb in range(2) for b in range(2)])
    return nc, P


def host_scan2(streams):
    T = TSEQ
    nc, P = cached("scan2", build_scan2, T)
    consts = scan2_consts()
    maps = []
    for h in range(8):
        s = streams[h]
        m = dict(consts)
        for dn, idx in enumerate((IDX_F, IDX_B)):
            arr = np.stack([s["kk"], s[f"nb{dn}"], s[f"kd{dn}"], s["r"], s[f"s{dn}"], s["v"]])
            m[f"in{dn}"] = np.ascontiguousarray(arr[:, :, idx])
        maps.append(m)
    res = run_prog(nc, maps)
    out = []
    for h in range(8):
        ys = []
        for dn, idx in enumerate((IDX_F, IDX_B)):
            y = res[h][f"y{dn}"]
            yfm = np.empty((128, T), np.float32)
            for b in range(2):
                yfm[b * 64:(b + 1) * 64][:, idx] = y[b].T
            ys.append(yfm)
        out.append(tuple(ys))
    return out
```
